# Optimizing a Trainium2 kernel written in Bass

```python
import math
import jax, jax.numpy as jnp
from jax import lax
import numpy as np

D_MODEL = 2048
BATCH = 4
SEQ = 4096
DEPTH = 1

CTX_LEN = 256
GRID_W = 64
EPS = 1e-6
N_MOD = 6
S5_WIDTH = 1024
S5_GROUP = 16
S5_GROUPS = S5_WIDTH // S5_GROUP
S5_STATE = 64
POOL_WIDTH = 1024
POOL_WINDOWS = (2, 4, 8, 16)
POOL_GROUPS = len(POOL_WINDOWS)
POOL_GROUP_W = POOL_WIDTH // POOL_GROUPS
N_BRANCH = 2
IN_WIDTH = S5_WIDTH + POOL_WIDTH + N_BRANCH * D_MODEL
PEER_HEADS = 8
PEER_KEYS = 128
PEER_EXPERTS = PEER_KEYS * PEER_KEYS
PEER_QDIM = 256
PEER_HALF = PEER_QDIM // 2
PEER_TOPK = 16
PEER_BLOCK = 128

kernel_name = "hybrid_s5_pool_peer_dit_block"


def rmsnorm(x, w):
    xf = x.astype(jnp.float32)
    y = xf * lax.rsqrt(jnp.mean(xf * xf, axis=-1, keepdims=True) + EPS)
    return (y * w.astype(jnp.float32)).astype(x.dtype)


def modulate(x, w, shift, scale):
    return rmsnorm(x, w) * (1 + scale) + shift


def s5_discretise(a_re, a_im, log_dt, b_re, b_im):
    a_re = a_re.astype(jnp.float32)
    a_im = a_im.astype(jnp.float32)
    b_re = b_re.astype(jnp.float32)
    b_im = b_im.astype(jnp.float32)
    dt = jnp.exp(log_dt.astype(jnp.float32))[:, None]
    mag = jnp.exp(dt * a_re)
    ab_re = mag * jnp.cos(dt * a_im)
    ab_im = mag * jnp.sin(dt * a_im)
    den = a_re * a_re + a_im * a_im
    nr = ab_re - 1.0
    ni = ab_im
    f_re = (nr * a_re + ni * a_im) / den
    f_im = (ni * a_re - nr * a_im) / den
    bb_re = f_re[..., None] * b_re - f_im[..., None] * b_im
    bb_im = f_re[..., None] * b_im + f_im[..., None] * b_re
    return ab_re, ab_im, bb_re, bb_im


def _complex_affine_combine(e1, e2):
    a1r, a1i, b1r, b1i = e1
    a2r, a2i, b2r, b2i = e2
    return (a2r * a1r - a2i * a1i,
            a2r * a1i + a2i * a1r,
            a2r * b1r - a2i * b1i + b2r,
            a2r * b1i + a2i * b1r + b2i)


def s5_scan(u, ab_re, ab_im, bb_re, bb_im, reverse, h0_re=None, h0_im=None):
    L = u.shape[1]
    b_re = jnp.einsum('blgh,gph->blgp', u, bb_re)
    b_im = jnp.einsum('blgh,gph->blgp', u, bb_im)
    a_re = jnp.broadcast_to(ab_re[None, None], (1, L) + ab_re.shape)
    a_im = jnp.broadcast_to(ab_im[None, None], (1, L) + ab_im.shape)
    p_re, p_im, h_re, h_im = lax.associative_scan(
        _complex_affine_combine, (a_re, a_im, b_re, b_im), reverse=reverse, axis=1)
    if h0_re is not None:
        h0_re = h0_re[:, None]
        h0_im = h0_im[:, None]
        h_re, h_im = (h_re + p_re * h0_re - p_im * h0_im,
                      h_im + p_re * h0_im + p_im * h0_re)
    return h_re, h_im


def s5_readout(h_re, h_im, c_re, c_im):
    return (jnp.einsum('blgp,ghp->blgh', h_re, c_re.astype(jnp.float32))
            - jnp.einsum('blgp,ghp->blgh', h_im, c_im.astype(jnp.float32)))


def s5_output(y, u, d_skip, w_glu, b_glu, dtype):
    Bn, L = u.shape[:2]
    y = y.reshape(Bn, L, S5_WIDTH) + d_skip.astype(jnp.float32) * u.reshape(Bn, L, S5_WIDTH)
    g = jax.nn.gelu(y)
    return (g * jax.nn.sigmoid(g @ w_glu.astype(jnp.float32) + b_glu.astype(jnp.float32))).astype(dtype)


def s5_branch(u_lat, u_ctx, a_re, a_im, log_dt, b_re, b_im, c_re, c_im, d_skip, w_glu, b_glu, need_ctx):
    Bn, L, _ = u_lat.shape
    Lc = u_ctx.shape[1]
    ul = u_lat.astype(jnp.float32).reshape(Bn, L, S5_GROUPS, S5_GROUP)
    uc = u_ctx.astype(jnp.float32).reshape(Bn, Lc, S5_GROUPS, S5_GROUP)
    y_lat = 0.0
    y_ctx = 0.0
    for d, rev in enumerate((False, True)):
        ab_re, ab_im, bb_re, bb_im = s5_discretise(a_re[d], a_im[d], log_dt[d], b_re[d], b_im[d])
        hc_re, hc_im = s5_scan(uc, ab_re, ab_im, bb_re, bb_im, rev)
        end = 0 if rev else Lc - 1
        hl_re, hl_im = s5_scan(ul, ab_re, ab_im, bb_re, bb_im, rev, hc_re[:, end], hc_im[:, end])
        y_lat = y_lat + s5_readout(hl_re, hl_im, c_re[d], c_im[d])
        if need_ctx:
            y_ctx = y_ctx + s5_readout(hc_re, hc_im, c_re[d], c_im[d])
    out_lat = s5_output(y_lat, ul, d_skip, w_glu, b_glu, u_lat.dtype)
    out_ctx = s5_output(y_ctx, uc, d_skip, w_glu, b_glu, u_ctx.dtype) if need_ctx else None
    return out_lat, out_ctx


def pool_branch(u, n_seg, seg_len, pool_w, pool_scale):
    Bn, L, _ = u.shape
    us = u.astype(jnp.float32).reshape(Bn, n_seg, seg_len, POOL_GROUPS, POOL_GROUP_W)
    cs = jnp.pad(jnp.cumsum(us, axis=2), ((0, 0), (0, 0), (1, 0), (0, 0), (0, 0)))
    pos = jnp.arange(seg_len)
    outs = []
    for g, w in enumerate(POOL_WINDOWS):
        lo = jnp.clip(pos - w // 2, 0, seg_len - 1)
        hi = jnp.clip(pos + w // 2 - 1, 0, seg_len - 1)
        cs_g = cs[:, :, :, g]
        win_sum = jnp.take(cs_g, hi + 1, axis=2) - jnp.take(cs_g, lo, axis=2)
        cnt = (hi - lo + 1).astype(jnp.float32)[:, None]
        outs.append(win_sum / cnt - us[:, :, :, g])
    p = jnp.stack(outs, axis=3)
    y = jnp.einsum('bnlgc,gcd->bnlgd', p, pool_w.astype(jnp.float32))
    return (y.reshape(Bn, L, POOL_WIDTH) * pool_scale.astype(jnp.float32)).astype(u.dtype)


def merge_branches(z, y_a, y_b, proj_a, proj_b, w_out):
    g0 = S5_WIDTH + POOL_WIDTH
    gate_a = jax.nn.sigmoid(z[..., g0:g0 + D_MODEL])
    gate_b = jax.nn.sigmoid(z[..., g0 + D_MODEL:g0 + 2 * D_MODEL])
    return (gate_a * (y_a @ proj_a) + gate_b * (y_b @ proj_b)) @ w_out


def peer_ffn(h, w_q, k1, k2, u_tab, v_tab):
    Bn, L, Dm = h.shape
    blocks = h.reshape((Bn * L) // PEER_BLOCK, PEER_BLOCK, Dm)

    def block_fn(hb):
        q = (hb @ w_q).reshape(PEER_BLOCK, PEER_HEADS, 2, PEER_HALF)
        s1 = jnp.einsum('thd,kd->thk', q[:, :, 0], k1).astype(jnp.float32)
        s2 = jnp.einsum('thd,kd->thk', q[:, :, 1], k2).astype(jnp.float32)
        v1, i1 = lax.top_k(s1, PEER_TOPK)
        v2, i2 = lax.top_k(s2, PEER_TOPK)
        cand = (v1[..., :, None] + v2[..., None, :]).reshape(PEER_BLOCK, PEER_HEADS, PEER_TOPK * PEER_TOPK)
        cid = (i1[..., :, None] * PEER_KEYS + i2[..., None, :]).reshape(PEER_BLOCK, PEER_HEADS, PEER_TOPK * PEER_TOPK)
        top_s, pos = lax.top_k(cand, PEER_TOPK)
        eid = jnp.take_along_axis(cid, pos, axis=-1)
        g = jax.nn.softmax(top_s, axis=-1)
        u_sel = jnp.take(u_tab, eid, axis=0)
        act = jax.nn.gelu(jnp.einsum('td,thkd->thk', hb, u_sel).astype(jnp.float32)) * g
        v_sel = jnp.take(v_tab, eid, axis=0)
        return jnp.einsum('thk,thkd->td', act.astype(hb.dtype), v_sel)

    return lax.map(block_fn, blocks).reshape(Bn, L, Dm)


def setup_inputs(seed: int = 0) -> dict:
    key = jax.random.key(seed)
    ks = jax.random.split(key, 32)

    def nrm(k, shape, s):
        return s * jax.random.normal(k, shape, jnp.float32)

    Ld, G, P, H = DEPTH, S5_GROUPS, S5_STATE, S5_GROUP
    n = jnp.arange(P, dtype=jnp.float32)
    return {
        "x": nrm(ks[0], (BATCH, SEQ, D_MODEL), 1.0),
        "c": nrm(ks[1], (BATCH, D_MODEL), 1.0),
        "ctx": nrm(ks[2], (BATCH, CTX_LEN, D_MODEL), 1.0),
        "c_ctx": nrm(ks[3], (D_MODEL,), 1.0),
        "w_mod": nrm(ks[4], (Ld, D_MODEL, N_MOD * D_MODEL), 0.5 * D_MODEL ** -0.5),
        "b_mod": nrm(ks[5], (Ld, N_MOD * D_MODEL), 0.02),
        "norm1_w": 1.0 + nrm(ks[6], (Ld, D_MODEL), 0.02),
        "norm2_w": 1.0 + nrm(ks[7], (Ld, D_MODEL), 0.02),
        "w_in": nrm(ks[8], (Ld, D_MODEL, IN_WIDTH), D_MODEL ** -0.5),
        "s5_a_re": -0.5 + nrm(ks[9], (Ld, 2, G, P), 0.01),
        "s5_a_im": math.pi * n + nrm(ks[10], (Ld, 2, G, P), 0.01),
        "s5_log_dt": jax.random.uniform(ks[11], (Ld, 2, G), jnp.float32, math.log(1e-3), math.log(1e-1)),
        "s5_b_re": nrm(ks[12], (Ld, 2, G, P, H), (2 * H) ** -0.5),
        "s5_b_im": nrm(ks[13], (Ld, 2, G, P, H), (2 * H) ** -0.5),
        "s5_c_re": nrm(ks[14], (Ld, 2, G, H, P), (2 * P) ** -0.5),
        "s5_c_im": nrm(ks[15], (Ld, 2, G, H, P), (2 * P) ** -0.5),
        "s5_d": nrm(ks[16], (Ld, S5_WIDTH), 0.5),
        "w_glu": nrm(ks[17], (Ld, S5_WIDTH, S5_WIDTH), S5_WIDTH ** -0.5),
        "b_glu": nrm(ks[18], (Ld, S5_WIDTH), 0.02),
        "pool_w": nrm(ks[19], (Ld, POOL_GROUPS, POOL_GROUP_W, POOL_GROUP_W), POOL_GROUP_W ** -0.5),
        "pool_scale": 1.0 + nrm(ks[20], (Ld, POOL_WIDTH), 0.05),
        "proj_a": nrm(ks[21], (Ld, S5_WIDTH, D_MODEL), S5_WIDTH ** -0.5),
        "proj_b": nrm(ks[22], (Ld, POOL_WIDTH, D_MODEL), POOL_WIDTH ** -0.5),
        "w_out": nrm(ks[23], (Ld, D_MODEL, D_MODEL), D_MODEL ** -0.5),
        "peer_wq": nrm(ks[24], (Ld, D_MODEL, PEER_HEADS * PEER_QDIM), D_MODEL ** -0.5),
        "peer_k1": nrm(ks[25], (Ld, PEER_KEYS, PEER_HALF), PEER_HALF ** -0.5),
        "peer_k2": nrm(ks[26], (Ld, PEER_KEYS, PEER_HALF), PEER_HALF ** -0.5),
        "peer_u": nrm(ks[27], (Ld, PEER_EXPERTS, D_MODEL), D_MODEL ** -0.5),
        "peer_v": nrm(ks[28], (Ld, PEER_EXPERTS, D_MODEL), 0.5),
        "final_w": 1.0 + nrm(ks[29], (D_MODEL,), 0.02),
    }


def reference(x, c, ctx, c_ctx, w_mod, b_mod, norm1_w, norm2_w, w_in,
              s5_a_re, s5_a_im, s5_log_dt, s5_b_re, s5_b_im, s5_c_re, s5_c_im, s5_d,
              w_glu, b_glu, pool_w, pool_scale, proj_a, proj_b, w_out,
              peer_wq, peer_k1, peer_k2, peer_u, peer_v, final_w):
    rows = x.shape[1] // GRID_W
    ctx_len = ctx.shape[1]
    for l in range(DEPTH):
        need_ctx = l < DEPTH - 1
        mod_lat = jax.nn.silu(c) @ w_mod[l] + b_mod[l]
        mod_ctx = jax.nn.silu(c_ctx) @ w_mod[l] + b_mod[l]
        sh1, sc1, g1, sh2, sc2, g2 = jnp.split(mod_lat[:, None, :], N_MOD, axis=-1)
        csh1, csc1, cg1, csh2, csc2, cg2 = jnp.split(mod_ctx, N_MOD, axis=-1)

        h = modulate(x, norm1_w[l], sh1, sc1)
        hc = modulate(ctx, norm1_w[l], csh1, csc1)
        z = h @ w_in[l]
        zc = hc @ w_in[l][:, :(IN_WIDTH if need_ctx else S5_WIDTH)]
        y_a, y_a_ctx = s5_branch(z[..., :S5_WIDTH], zc[..., :S5_WIDTH],
                                 s5_a_re[l], s5_a_im[l], s5_log_dt[l], s5_b_re[l], s5_b_im[l],
                                 s5_c_re[l], s5_c_im[l], s5_d[l], w_glu[l], b_glu[l], need_ctx)
        y_b = pool_branch(z[..., S5_WIDTH:S5_WIDTH + POOL_WIDTH], rows, GRID_W, pool_w[l], pool_scale[l])
        x = x + g1 * merge_branches(z, y_a, y_b, proj_a[l], proj_b[l], w_out[l])

        x = x + g2 * peer_ffn(modulate(x, norm2_w[l], sh2, sc2),
                              peer_wq[l], peer_k1[l], peer_k2[l], peer_u[l], peer_v[l])

        if need_ctx:
            y_b_ctx = pool_branch(zc[..., S5_WIDTH:S5_WIDTH + POOL_WIDTH], 1, ctx_len, pool_w[l], pool_scale[l])
            ctx = ctx + cg1 * merge_branches(zc, y_a_ctx, y_b_ctx, proj_a[l], proj_b[l], w_out[l])
            ctx = ctx + cg2 * peer_ffn(modulate(ctx, norm2_w[l], csh2, csc2),
                                       peer_wq[l], peer_k1[l], peer_k2[l], peer_u[l], peer_v[l])
    return rmsnorm(x, final_w)
```

```python
import contextlib
import math
import numpy as np
import concourse.bass as bass
import concourse.mybir as mybir
from concourse.bass_utils import run_bass_kernel_spmd

F32 = mybir.dt.float32
BF16 = mybir.dt.bfloat16
U32 = mybir.dt.uint32
ALU = mybir.AluOpType
AF = mybir.ActivationFunctionType
AX = mybir.AxisListType
AP = bass.AP

D = 2048
SEQ = 4096
CTX = 256
NOWN = 2048
TSEQ = SEQ + CTX
EPS = 1e-6
TWO_PI = 2.0 * math.pi
TWO_PI_S = 6.28318
NEG_BIG = -1.0e30
MAGIC = 12582912.0
INV_2PI = 1.0 / (2.0 * math.pi)


def ap_of(t):
    return t[:] if not isinstance(t, AP) else t


def mk_ap(base, offset_elems, dims):
    b = ap_of(base)
    return AP(b.tensor, b.offset + offset_elems, [list(b.ap[0])] + [list(d) for d in dims])


class K:
    def __init__(self, nc, es):
        self.nc = nc
        self.es = es
        self.E = {"pe": nc.tensor, "dve": nc.vector, "act": nc.scalar, "pool": nc.gpsimd, "sp": nc.sync}
        self.tok = {}
        for e in ("pe", "dve", "act", "pool"):
            self.tok[e] = dict(sem=es.enter_context(nc.semaphore("s_" + e)), cnt=0, step=1)
        self.dq = {}
        self.qeng = {"sp": "sp", "pool": "pool", "act": "act", "cv": "pool"}
        self.keep = set()
        for q, n in (("sp", 16), ("pool", 8), ("act", 4), ("cv", 4)):
            names = []
            for i in range(n):
                nm = "d_%s%d" % (q, i)
                self.tok[nm] = dict(sem=es.enter_context(nc.semaphore(nm)), cnt=0, step=16)
                names.append(nm)
            self.dq[q] = [names, 0]
        self.waited = {e: {} for e in self.E}
        self.lastw = {}
        self.reads = {}
        self.ninst = 0

    def sb(self, es, name, shape, dt):
        self.ninst += 0
        self._uid = getattr(self, "_uid", 0) + 1
        return es.enter_context(self.nc.sbuf_tensor("sb%d_%s" % (self._uid, name), list(shape), dt))

    def ps(self, es, name, shape, dt=F32):
        self._uid = getattr(self, "_uid", 0) + 1
        return es.enter_context(self.nc.psum_tensor("ps%d_%s" % (self._uid, name), list(shape), dt))

    def _deps(self, reads, writes):
        deps = {}

        def add(tv):
            if tv is None:
                return
            t, v = tv
            if deps.get(t, 0) < v:
                deps[t] = v

        for k in reads:
            add(self.lastw.get(k))
        for k in writes:
            add(self.lastw.get(k))
            for t, v in self.reads.get(k, {}).items():
                add((t, v))
        return deps

    def _wait(self, e, deps):
        for t, v in deps.items():
            if t == e and e == "pe":
                continue
            if self.waited[e].get(t, 0) >= v:
                continue
            tk = self.tok[t]
            self.E[e].wait_ge(tk["sem"], v * tk["step"])
            self.waited[e][t] = v

    def _record(self, t, v, reads, writes):
        for k in reads:
            r = self.reads.setdefault(k, {})
            if r.get(t, 0) < v:
                r[t] = v
        for k in writes:
            self.lastw[k] = (t, v)
            self.reads[k] = {}

    def op(self, e, fn, reads=(), writes=(), sig=True):
        deps = self._deps(reads, writes)
        self._wait(e, deps)
        inst = fn(self.E[e])
        tk = self.tok[e]
        if sig:
            inst.then_inc(tk["sem"], 1)
            tk["cnt"] += 1
            v = tk["cnt"]
        else:
            v = tk["cnt"] + 1
        self._record(e, v, reads, writes)
        self.ninst += 1
        return inst

    def dma(self, q, out, in_, reads=(), writes=(), idx=None):
        names, rr = self.dq[q]
        nm = names[rr % len(names)]
        self.dq[q][1] += 1
        tk = self.tok[nm]
        deps = self._deps(reads, writes)
        if tk["cnt"] > 0 and deps.get(nm, 0) < tk["cnt"]:
            deps[nm] = tk["cnt"]
        qe = self.qeng[q]
        self._wait(qe, deps)
        if idx is None:
            inst = self.E[qe].dma_start(out=out, in_=in_)
        else:
            inst = self.E[qe].indirect_dma_start(
                out=out, out_offset=None, in_=in_,
                in_offset=bass.IndirectOffsetOnAxis(ap=idx, axis=0))
        inst.then_inc(tk["sem"], 16)
        tk["cnt"] += 1
        self._record(nm, tk["cnt"], reads, writes)
        self.ninst += 1
        return inst

    def barrier(self, include_cv=False):
        for e in self.E:
            deps = {t: tk["cnt"] for t, tk in self.tok.items()
                    if tk["cnt"] > 0 and (include_cv or not t.startswith("d_cv"))}
            self._wait(e, deps)
        self.lastw = {k_: v for k_, v in self.lastw.items() if k_ in self.keep}
        self.reads = {}

    def final_wait(self):
        deps = {t: tk["cnt"] for t, tk in self.tok.items() if tk["cnt"] > 0}
        self._wait("sp", deps)

    def mm(self, out, lhsT, rhs, start, stop, reads, writes, sig=None):
        if sig is None:
            sig = stop
        return self.op("pe", lambda e: e.matmul(out, lhsT, rhs, start=start, stop=stop),
                       reads=reads, writes=writes, sig=sig)

    def tr(self, out, in_, ident, reads, writes, sig=True):
        return self.op("pe", lambda e: e.transpose(out, in_, ident), reads=reads, writes=writes, sig=sig)

    def tt(self, e, out, in0, in1, op, reads, writes):
        return self.op(e, lambda g: g.tensor_tensor(out=out, in0=in0, in1=in1, op=op), reads=reads, writes=writes)

    def ts(self, e, out, in0, s1, s2, op0, op1, reads, writes):
        if op1 is None:
            return self.op(e, lambda g: g.tensor_scalar(out=out, in0=in0, scalar1=s1, scalar2=None, op0=op0),
                           reads=reads, writes=writes)
        return self.op(e, lambda g: g.tensor_scalar(out=out, in0=in0, scalar1=s1, scalar2=s2, op0=op0, op1=op1),
                       reads=reads, writes=writes)

    def stt(self, out, in0, scalar, in1, op0, op1, reads, writes, accum_out=None):
        if accum_out is None:
            return self.op("dve", lambda g: g.scalar_tensor_tensor(out=out, in0=in0, scalar=scalar, in1=in1,
                                                                    op0=op0, op1=op1), reads=reads, writes=writes)
        return self.op("dve", lambda g: g.scalar_tensor_tensor(out=out, in0=in0, scalar=scalar, in1=in1,
                                                                op0=op0, op1=op1, accum_out=accum_out),
                       reads=reads, writes=writes)

    def cp(self, e, out, in_, reads, writes):
        return self.op(e, lambda g: g.tensor_copy(out=out, in_=in_), reads=reads, writes=writes)

    def act(self, out, in_, func, reads, writes, bias=None, scale=None, accum_out=None):
        kw = {}
        if bias is not None:
            kw["bias"] = bias
        if scale is not None:
            kw["scale"] = scale
        if accum_out is not None:
            kw["accum_out"] = accum_out
        return self.op("act", lambda g: g.activation(out=out, in_=in_, func=func, **kw), reads=reads, writes=writes)

    def memset(self, e, ap, val, writes):
        return self.op(e, lambda g: g.memset(ap, val), reads=(), writes=writes)


def build_program(stop_after="E", dbg=False):
    nc = bass.Bass("TRN2", target_bir_lowering=False)
    phases = "ABCDE"
    last = phases.index(stop_after)

    def din(name, shape, dt=F32):
        return nc.dram_tensor(name, list(shape), dt, kind="ExternalInput").ap()

    def dscr(name, shape, dt):
        return nc.dram_tensor(name, list(shape), dt, kind=("ExternalOutput" if dbg else "Internal")).ap()

    def dout(name, shape, dt=F32):
        return nc.dram_tensor(name, list(shape), dt, kind="ExternalOutput").ap()

    xs = din("xs", [TSEQ, D])
    cT = din("cT", [128, 32])
    w_mod = din("w_mod", [D, 6 * D])
    bmod_bc = din("bmod_bc", [128, 6 * D])
    n1T = din("n1T", [128, 16])
    n2_bc = din("n2_bc", [128, D])
    fw_bc = din("fw_bc", [128, D])
    w_in = din("w_in", [D, 6144])
    ident_in = din("ident", [128, 128])
    pm2_in = din("pm2", [128, 4 * 128])
    poolw_in = din("poolw", [1024, 256])
    pscT = din("pscT", [128, 8])
    e0_in = din("e0", [128, 1])
    if last >= 2:
        tidx_in = din("tidx", [128, TSEQ])
        s5_arep = din("s5_arep", [128, 128])
        s5_aimp = din("s5_aimp", [128, 128])
        s5_ldtp = din("s5_ldtp", [128, 128])
        s5_aref = din("s5_aref", [128, 16 * 128])
        s5_aimf = din("s5_aimf", [128, 16 * 128])
        s5_ldtf = din("s5_ldtf", [128, 16 * 128])
        s5_brt = din("s5_brt", [128, 16 * 128])
        s5_bst = din("s5_bst", [128, 16 * 128])
        s5_l1 = din("s5_l1", [128, 128 * 16])
        s5_l2 = din("s5_l2", [128, 128 * 16])
        sgn_in = din("sgn", [128, 4 + 128])
        gmask_in = din("gmask", [128, 8])
        dskT = din("dskT", [128, 8])
        bgluT = din("bgluT", [128, 8])
        w_glu = din("w_glu", [1024, 1024])
    if last >= 3:
        proj_a = din("proj_a", [1024, D])
        proj_b = din("proj_b", [1024, D])
        w_out = din("w_out", [D, D])
    if last >= 4:
        wq = din("wq", [D, D])
        k1T = din("k1T", [128, 128])
        k2T = din("k2T", [128, 128])
        peer_u = din("peer_u", [16384, D])
        peer_v = din("peer_v", [16384, D])
        iota16_in = din("iota16", [128, 16])
        y_out = dout("y", [NOWN, D])

    uT_d = dscr("uT_d", [8, 128, TSEQ], BF16)
    rows_d = dscr("rows_d", [4, 128, D], F32)
    ybT_d = dscr("ybT_d", [8, 128, NOWN], BF16)
    hT_d = nc.dram_tensor("hT_d", [16, 128, NOWN], BF16, kind="Internal").ap()
    if last >= 3:
        x1_d = dscr("x1_d", [NOWN, D], F32)
        h2b_d = dscr("h2b_d", [NOWN, D], BF16)
    if last >= 3:
        w16g_d = nc.dram_tensor("w16g_d", [D, 4096], BF16, kind="Internal").ap()
        pa16_d = nc.dram_tensor("pa16_d", [1024, D], BF16, kind="Internal").ap()
        pb16_d = nc.dram_tensor("pb16_d", [1024, D], BF16, kind="Internal").ap()
        wo16_d = nc.dram_tensor("wo16_d", [D, D], BF16, kind="Internal").ap()
    if last >= 4:
        uv16_d = nc.dram_tensor("uv16_d", [16384, 2 * D], BF16, kind="Internal").ap()
    if dbg:
        dbg_feat = dout("dbg_feat", [128, 64])
        dbg_rows = dout("dbg_rows", [4, D])
        if last >= 2:
            dbg_ya = dout("dbg_ya", [8, 128, NOWN], BF16)
            dbg_g = dout("dbg_g", [8, 128, NOWN], BF16)

    es = contextlib.ExitStack()
    with es:
        k = K(nc, es)
        feat = k.sb(es, "feat", [128, 64], F32)
        wm1 = k.sb(es, "wm1", [128, 32], F32)
        ident_f = k.sb(es, "ident_f", [128, 128], F32)
        ident_b = k.sb(es, "ident_b", [128, 128], BF16)
        negpi = k.sb(es, "negpi", [128, 1], F32)
        e0 = k.sb(es, "e0", [128, 1], F32)
        n1Ts = k.sb(es, "n1Ts", [128, 16], F32)

        k.dma("sp", ident_f[:], ident_in[:, :], writes=["ident_f"])
        k.dma("sp", e0[:], e0_in[:, :], writes=["e0"])
        k.dma("sp", n1Ts[:], n1T[:, :], writes=["n1Ts"])
        k.cp("dve", ident_b[:], ident_f[:], reads=["ident_f"], writes=["ident_b"])
        k.memset("dve", negpi[:], -math.pi, writes=["negpi"])
        halfpi = k.sb(es, "halfpi", [128, 1], F32)
        k.memset("dve", halfpi[:], math.pi / 2, writes=["halfpi"])
        epsc = k.sb(es, "epsc", [128, 1], F32)
        k.memset("dve", epsc[:], EPS, writes=["epsc"])
        magp = k.sb(es, "magp", [128, 1], F32)
        magn = k.sb(es, "magn", [128, 1], F32)
        k.memset("dve", magp[:], MAGIC, writes=["magp"])
        k.memset("dve", magn[:], -MAGIC, writes=["magn"])

        with contextlib.ExitStack() as pa:
            cTs = k.sb(pa, "cTs", [128, 32], F32)
            sil = k.sb(pa, "sil", [128, 32], F32)
            silbc = k.sb(pa, "silbc", [128, 32 * 128], F32)
            NWA = 4
            wst = [k.sb(pa, "wst%d" % i, [128, 4096], F32) for i in range(NWA)]
            bst = k.sb(pa, "bst", [128, 4096], F32)
            rowt = k.sb(pa, "rowt", [128, 4096], F32)
            psA = k.ps(pa, "psA", [128, 4096], F32)
            g1bc = k.sb(pa, "g1bc", [128, D], F32)
            sh2bc = k.sb(pa, "sh2bc", [128, D], F32)
            wm2bc = k.sb(pa, "wm2bc", [128, D], F32)
            g2bc = k.sb(pa, "g2bc", [128, D], F32)
            k.dma("sp", cTs[:], cT[:, :], writes=["cTs"])
            k.act(sil[:], cTs[:], AF.Silu, reads=["cTs"], writes=["sil"])
            k.cp("dve", mk_ap(silbc, 0, [[128, 32], [1, 128]]), mk_ap(sil, 0, [[1, 32], [0, 128]]),
                 reads=["sil"], writes=["silbc"])
            nld = 0
            for hh in range(2):
                c0 = hh * 2048
                k.dma("sp", bst[:, 0:2048], bmod_bc[:, c0:c0 + 2048], writes=["bst"])
                for kc in range(16):
                    wb = wst[nld % NWA]
                    wk = "wst%d" % (nld % NWA)
                    nld += 1
                    k.dma("sp", wb[:, 0:2048], w_mod[kc * 128:(kc + 1) * 128, c0:c0 + 2048], writes=[wk])
                    for v in range(2):
                        for nb in range(4):
                            bk = v * 4 + nb
                            k.mm(psA[:, bk * 512:(bk + 1) * 512],
                                 silbc[:, (v * 16 + kc) * 128:(v * 16 + kc + 1) * 128],
                                 wb[:, nb * 512:(nb + 1) * 512],
                                 start=(kc == 0), stop=(kc == 15),
                                 reads=[wk, "silbc"], writes=["psA%d" % bk], sig=(bk == 7))
                pkeys = ["psA%d" % nb for nb in range(8)]
                for v in range(2):
                    k.tt("dve", rowt[:, v * 2048:(v + 1) * 2048], psA[:, v * 2048:(v + 1) * 2048], bst[:, 0:2048],
                         ALU.add, reads=pkeys + ["bst"], writes=["rowt"])
                psF = psA
                for v in range(2):
                    for ch in range(16):
                        col = v * 32 + hh * 16 + ch
                        k.mm(psF[:, col:col + 1], rowt[:, v * 2048 + ch * 128:v * 2048 + (ch + 1) * 128], e0[:, 0:1],
                             start=True, stop=True, reads=["rowt", "e0"], writes=["psA0"],
                             sig=(v == 1 and ch == 15))
                for v in range(2):
                    k.cp("dve", feat[:, v * 32 + hh * 16:v * 32 + hh * 16 + 16],
                         psF[:, v * 32 + hh * 16:v * 32 + hh * 16 + 16], reads=["psA0"], writes=["feat"])
            for v in range(2):
                k.stt(wm1[:, v * 16:(v + 1) * 16], feat[:, v * 32 + 16:v * 32 + 32], 1.0, n1Ts[:],
                      ALU.add, ALU.mult, reads=["feat", "n1Ts"], writes=["wm1"])
            passes = [(1, 0), (2, 0)]
            for pi, (th, v) in enumerate(passes):
                c0 = th * 4096
                k.dma("sp", bst[:], bmod_bc[:, c0:c0 + 4096], writes=["bst"])
                for kc in range(16):
                    wb = wst[nld % NWA]
                    wk = "wst%d" % (nld % NWA)
                    nld += 1
                    k.dma("sp", wb[:], w_mod[kc * 128:(kc + 1) * 128, c0:c0 + 4096], writes=[wk])
                    for nb in range(8):
                        k.mm(psA[:, nb * 512:(nb + 1) * 512],
                             silbc[:, (v * 16 + kc) * 128:(v * 16 + kc + 1) * 128],
                             wb[:, nb * 512:(nb + 1) * 512],
                             start=(kc == 0), stop=(kc == 15),
                             reads=[wk, "silbc"], writes=["psA%d" % nb], sig=(nb == 7))
                pkeys = ["psA%d" % nb for nb in range(8)]
                if th == 0:
                    pass
                elif th == 1:
                    k.tt("dve", g1bc[:], psA[:, 0:2048], bst[:, 0:2048], ALU.add,
                         reads=pkeys + ["bst"], writes=["g1bc"])
                    k.tt("dve", sh2bc[:], psA[:, 2048:4096], bst[:, 2048:4096], ALU.add,
                         reads=pkeys + ["bst"], writes=["sh2bc"])
                else:
                    k.tt("dve", rowt[:, 0:2048], psA[:, 0:2048], bst[:, 0:2048], ALU.add,
                         reads=pkeys + ["bst"], writes=["rowt"])
                    k.tt("dve", g2bc[:], psA[:, 2048:4096], bst[:, 2048:4096], ALU.add,
                         reads=pkeys + ["bst"], writes=["g2bc"])
                    k.dma("sp", bst[:, 0:2048], n2_bc[:, :], reads=[], writes=["bst"])
                    k.stt(wm2bc[:], rowt[:, 0:2048], 1.0, bst[:, 0:2048], ALU.add, ALU.mult,
                          reads=["rowt", "bst"], writes=["wm2bc"])
            for ri_, (t_, tk_) in enumerate(((g1bc, "g1bc"), (sh2bc, "sh2bc"), (wm2bc, "wm2bc"), (g2bc, "g2bc"))):
                k.dma("sp", rows_d[ri_, :, :], t_[:], reads=[tk_], writes=["rows_d"])
            if dbg:
                k.dma("sp", dbg_feat[:, :], feat[:], reads=["feat"])
                k.dma("sp", dbg_rows[0:1, :], g1bc[0:1, :], reads=["g1bc"])
                k.dma("sp", dbg_rows[1:2, :], sh2bc[0:1, :], reads=["sh2bc"])
                k.dma("sp", dbg_rows[2:3, :], wm2bc[0:1, :], reads=["wm2bc"])
                k.dma("sp", dbg_rows[3:4, :], g2bc[0:1, :], reads=["g2bc"])
            k.barrier()

        def norm_tile(src_rows, xt, xtk, ss, rstd, xn, pT, hT_dst, vsel, junk):
            k.dma("sp", xt[:], src_rows, writes=[xtk])
            k.act(junk[:], xt[:], AF.Square, reads=[xtk], writes=["junk", "ss"], accum_out=ss[:, 0:1])
            k.act(rstd[:, 0:1], ss[:, 0:1], AF.Ln, reads=["ss", "epsc"], writes=["rstd"], scale=1.0 / D, bias=epsc[:, 0:1])
            k.act(rstd[:, 0:1], rstd[:, 0:1], AF.Exp, reads=["rstd"], writes=["rstd"], scale=-0.5)
            k.act(xn[:], xt[:], AF.Copy, reads=[xtk, "rstd"], writes=["xn"], scale=rstd[:, 0:1])
            for kc in range(16):
                k.tr(pT[:, kc * 128:(kc + 1) * 128], xn[:, kc * 128:(kc + 1) * 128], ident_b[:],
                     reads=["xn", "ident_b"], writes=["pT"], sig=(kc == 15))
            for kc in range(16):
                dst, dk = hT_dst(kc)
                k.act(dst, pT[:, kc * 128:(kc + 1) * 128], AF.Identity, reads=["pT", "wm1", "feat"], writes=[dk],
                      scale=wm1[:, vsel * 16 + kc:vsel * 16 + kc + 1],
                      bias=feat[:, vsel * 32 + kc:vsel * 32 + kc + 1])

        def load_cast(pool_es, dst_bf, dkey, src_ap, ncols, stage, skeys, cnt):
            sb_ = stage[cnt[0] % len(stage)]
            sk = skeys[cnt[0] % len(stage)]
            cnt[0] += 1
            k.dma("sp", sb_[:, 0:ncols], src_ap, writes=[sk])
            if cnt[0] % 2 == 0:
                k.act(dst_bf, sb_[:, 0:ncols], AF.Copy, reads=[sk], writes=[dkey])
            else:
                k.cp("dve", dst_bf, sb_[:, 0:ncols], reads=[sk], writes=[dkey])

        if last >= 1:
            with contextlib.ExitStack() as pb:
                win = k.sb(pb, "win", [128, 16 * 2048], BF16)
                stage = [k.sb(pb, "stg%d" % i, [128, 2048], F32) for i in range(4)]
                skeys = ["stg%d" % i for i in range(4)]
                cnt = [0]
                poolw = k.sb(pb, "poolw", [128, 8 * 256], BF16)
                pm2 = k.sb(pb, "pm2", [128, 4 * 128], BF16)
                psc = k.sb(pb, "psc", [128, 8], F32)
                xt = [k.sb(pb, "xt%d" % i, [128, D], F32) for i in range(2)]
                junk = k.sb(pb, "junk", [128, D], BF16)
                ss = k.sb(pb, "ss", [128, 1], F32)
                rstd = k.sb(pb, "rstd", [128, 1], F32)
                xn = k.sb(pb, "xn", [128, D], BF16)
                hTs = [k.sb(pb, "hT%d" % i, [128, 16 * 512], BF16) for i in range(2)]
                ublk = k.sb(pb, "ublk", [128, 8 * 512], BF16)
                uptm = k.sb(pb, "uptm", [128, 1024], BF16)
                pTs = k.sb(pb, "pTs", [128, 8 * 128], BF16)
                yblk = k.sb(pb, "yblk", [128, 8 * 512], BF16)
                pT = k.ps(pb, "pT", [128, D], BF16)
                pU = [k.ps(pb, "pU%d" % i, [128, 512], F32) for i in range(2)]
                pP = k.ps(pb, "pP", [128, 1024], F32)
                pQ = k.ps(pb, "pQ", [128, 1024], F32)

                for kc in range(16):
                    load_cast(pb, win[:, kc * 2048:(kc + 1) * 2048], "win", w_in[kc * 128:(kc + 1) * 128, 0:2048],
                              2048, stage, skeys, cnt)
                for r in range(8):
                    load_cast(pb, poolw[:, r * 256:(r + 1) * 256], "poolw", poolw_in[r * 128:(r + 1) * 128, :],
                              256, stage, skeys, cnt)
                load_cast(pb, pm2[:], "pm2", pm2_in[:, :], 512, stage, skeys, cnt)
                k.dma("sp", psc[:], pscT[:, :], writes=["psc"])
                k.barrier()

                blocks = [(SEQ, 256, 1, False)] + [(i * 512, 512, 0, i < 4) for i in range(8)]
                nx = 0
                nu = 0
                for bi_, (row0, ntok, vsel, own) in enumerate(blocks):
                    ntile = ntok // 128
                    hT = hTs[bi_ % 2]
                    hk = "hT%d" % (bi_ % 2)
                    for j in range(ntile):
                        xb = xt[nx % 2]
                        xk = "xt%d" % (nx % 2)
                        nx += 1
                        norm_tile(xs[row0 + j * 128:row0 + (j + 1) * 128, :], xb, xk, ss, rstd, xn, pT,
                                  lambda kc, j=j: (hT[:, kc * 512 + j * 128:kc * 512 + (j + 1) * 128], hk),
                                  vsel, junk)
                    for ct in range(8):
                        pu = pU[nu % 2]
                        puk = "pU%d" % (nu % 2)
                        nu += 1
                        for kc in range(16):
                            k.mm(pu[:, 0:ntok], win[:, kc * 2048 + ct * 128:kc * 2048 + (ct + 1) * 128],
                                 hT[:, kc * 512:kc * 512 + ntok], start=(kc == 0), stop=(kc == 15),
                                 reads=["win", hk], writes=[puk])
                        k.cp("dve", ublk[:, ct * 512:ct * 512 + ntok], pu[:, 0:ntok], reads=[puk], writes=["ublk"])
                    k.dma("sp", uT_d[:, :, row0:row0 + ntok].rearrange("c p t -> p c t"),
                          mk_ap(ublk, 0, [[512, 8], [1, ntok]]), reads=["ublk"], writes=["uT_d"])
                    if not own:
                        continue
                    k.dma("sp", hT_d[:, :, row0:row0 + 512].rearrange("c p t -> p c t"),
                          mk_ap(hT, 0, [[512, 16], [1, 512]]), reads=[hk], writes=["hT_d"])
                    for j in range(4):
                        for hf in range(2):
                            for kc in range(16):
                                k.mm(pP[:, hf * 512:(hf + 1) * 512], hT[:, kc * 512 + j * 128:kc * 512 + (j + 1) * 128],
                                     win[:, kc * 2048 + 1024 + hf * 512:kc * 2048 + 1024 + (hf + 1) * 512],
                                     start=(kc == 0), stop=(kc == 15), reads=["win", hk], writes=["pP%d" % hf])
                        k.act(uptm[:], pP[:], AF.Copy, reads=["pP0", "pP1"], writes=["uptm"])
                        for ct in range(8):
                            g = ct // 2
                            k.mm(pQ[:, ct * 128:(ct + 1) * 128], uptm[:, ct * 128:(ct + 1) * 128],
                                 pm2[:, g * 128:(g + 1) * 128], start=True, stop=True,
                                 reads=["uptm", "pm2"], writes=["pQ"], sig=(ct == 7))
                        k.cp("dve", pTs[:], pQ[:], reads=["pQ"], writes=["pTs"])
                        for cot in range(8):
                            g = cot // 2
                            hf = cot % 2
                            for cit in range(2):
                                k.mm(pQ[:, cot * 128:(cot + 1) * 128],
                                     poolw[:, (g * 2 + cit) * 256 + hf * 128:(g * 2 + cit) * 256 + (hf + 1) * 128],
                                     pTs[:, (g * 2 + cit) * 128:(g * 2 + cit + 1) * 128],
                                     start=(cit == 0), stop=(cit == 1), reads=["pTs", "poolw"], writes=["pQ"],
                                     sig=(cot == 7 and cit == 1))
                        k.tt("dve", mk_ap(yblk, j * 128, [[512, 8], [1, 128]]), mk_ap(pQ, 0, [[128, 8], [1, 128]]),
                             mk_ap(psc, 0, [[1, 8], [0, 128]]), ALU.mult, reads=["pQ", "psc"], writes=["yblk"])
                    k.dma("sp", ybT_d[:, :, row0:row0 + 512].rearrange("c p t -> p c t"),
                          mk_ap(yblk, 0, [[512, 8], [1, 512]]), reads=["yblk"], writes=["ybT_d"])
                k.barrier()

        if last >= 2:
            pcd = contextlib.ExitStack()
            es.callback(pcd.close)
            yaT = k.sb(pcd, "yaT", [128, 8 * NOWN], BF16)
            cv_jobs = []
            if last >= 3:
                k.keep.update(["w16"])
                for r_ in (0, 1024):
                    for ch_ in (0, 2048):
                        cv_jobs.append((w16g_d[r_:r_ + 1024, ch_:ch_ + 2048],
                                        w_in[r_:r_ + 1024, 2048 + ch_:2048 + ch_ + 2048], "w16"))
                cv_jobs.append((pa16_d[:, :], proj_a[:, :], "w16"))
                cv_jobs.append((pb16_d[:, :], proj_b[:, :], "w16"))
                for r_ in (0, 1024):
                    cv_jobs.append((wo16_d[r_:r_ + 1024, :], w_out[r_:r_ + 1024, :], "w16"))
            if last >= 4:
                CH = 1024
                k.keep.update(["u16_d", "v16_d"])
                for r_ in range(0, 16384, CH):
                    cv_jobs.append((uv16_d[r_:r_ + CH, 0:D], peer_u[r_:r_ + CH, :], "u16_d"))
                    cv_jobs.append((uv16_d[r_:r_ + CH, D:2 * D], peer_v[r_:r_ + CH, :], "v16_d"))
            with contextlib.ExitStack() as pc:
                B1b = k.sb(pc, "B1b", [128, 2048], BF16)
                B2b = k.sb(pc, "B2b", [128, 2048], BF16)
                rcb = k.sb(pc, "rcb", [128, 2048], BF16)
                rc2b = k.sb(pc, "rc2b", [128, 2048], BF16)
                Rp = k.sb(pc, "Rp", [128, 128], F32)
                w2p = k.sb(pc, "w2p", [128, 128], F32)
                gmask = k.sb(pc, "gmask", [128, 8], F32)
                gmaskn = k.sb(pc, "gmaskn", [128, 8], F32)
                dsk = k.sb(pc, "dsk", [128, 8], F32)
                bglu = k.sb(pc, "bglu", [128, 8], F32)
                k.dma("sp", gmask[:], gmask_in[:, :], writes=["gmask"])
                k.dma("sp", dsk[:], dskT[:, :], writes=["dsk"])
                k.dma("sp", bglu[:], bgluT[:, :], writes=["bglu"])
                k.ts("dve", gmaskn[:], gmask[:], -1.0, None, ALU.mult, None, reads=["gmask"], writes=["gmaskn"])
                with contextlib.ExitStack() as pp:
                    nm5 = ["aref", "aimf", "ldtf", "brt", "bst", "l1", "l2"]
                    T = {n: k.sb(pp, "c_" + n, [128, 2048], F32) for n in nm5}
                    for n, src in zip(nm5, [s5_aref, s5_aimf, s5_ldtf, s5_brt, s5_bst, s5_l1, s5_l2]):
                        k.dma("sp", T[n][:], src[:, :], writes=["c_" + n])
                    sgnf = k.sb(pp, "sgnf", [128, 128], F32)
                    sgn = k.sb(pp, "sgn", [128, 4], F32)
                    k.dma("sp", sgnf[:], sgn_in[:, 4:132], writes=["sgnf"])
                    k.dma("sp", sgn[:], sgn_in[:, 0:4], writes=["sgn"])
                    q = [k.sb(pp, "q%d" % i, [128, 128], F32) for i in range(4)]
                    for i_, src in enumerate([s5_arep, s5_aimp, s5_ldtp]):
                        k.dma("sp", q[i_][:], src[:, :], writes=["q%d" % i_])
                    k.act(q[3][:], q[2][:], AF.Exp, reads=["q2"], writes=["q3"])
                    k.tt("dve", q[0][:], q[3][:], q[0][:], ALU.mult, reads=["q3", "q0"], writes=["q0"])
                    k.act(Rp[:], q[0][:], AF.Exp, reads=["q0"], writes=["Rp"])
                    k.tt("dve", q[1][:], q[3][:], q[1][:], ALU.mult, reads=["q3", "q1"], writes=["q1"])
                    k.ts("dve", q[1][:], q[1][:], INV_2PI, None, ALU.mult, None, reads=["q1"], writes=["q1"])
                    k.ts("dve", q[2][:], q[1][:], MAGIC, MAGIC, ALU.add, ALU.subtract, reads=["q1"], writes=["q2"])
                    k.tt("dve", w2p[:], q[1][:], q[2][:], ALU.subtract, reads=["q1", "q2"], writes=["w2p"])
                    t = [k.sb(pp, "t%d" % i, [128, 2048], F32) for i in range(6)]
                    tk = ["t%d" % i for i in range(6)]
                    aref, aimf, ldtf, brt, bst_ = T["aref"], T["aimf"], T["ldtf"], T["brt"], T["bst"]
                    k.act(t[0][:], ldtf[:], AF.Exp, reads=["c_ldtf"], writes=[tk[0]])
                    k.tt("dve", t[1][:], t[0][:], aref[:], ALU.mult, reads=[tk[0], "c_aref"], writes=[tk[1]])
                    k.act(t[1][:], t[1][:], AF.Exp, reads=[tk[1]], writes=[tk[1]])
                    k.tt("dve", t[2][:], t[0][:], aimf[:], ALU.mult, reads=[tk[0], "c_aimf"], writes=[tk[2]])
                    k.ts("dve", t[2][:], t[2][:], INV_2PI, None, ALU.mult, None, reads=[tk[2]], writes=[tk[2]])
                    k.ts("dve", t[3][:], t[2][:], MAGIC, MAGIC, ALU.add, ALU.subtract, reads=[tk[2]], writes=[tk[3]])
                    k.tt("dve", t[2][:], t[2][:], t[3][:], ALU.subtract, reads=[tk[2], tk[3]], writes=[tk[2]])
                    k.act(t[3][:], t[2][:], AF.Sin, reads=[tk[2]], writes=[tk[3]], scale=TWO_PI_S)
                    k.act(t[2][:], t[2][:], AF.Abs, reads=[tk[2]], writes=[tk[2]])
                    k.act(t[2][:], t[2][:], AF.Sin, reads=[tk[2], "halfpi"], writes=[tk[2]], scale=-TWO_PI,
                          bias=halfpi[:, 0:1])
                    k.tt("dve", t[2][:], t[1][:], t[2][:], ALU.mult, reads=[tk[1], tk[2]], writes=[tk[2]])
                    k.tt("dve", t[3][:], t[1][:], t[3][:], ALU.mult, reads=[tk[1], tk[3]], writes=[tk[3]])
                    k.ts("dve", t[2][:], t[2][:], -1.0, None, ALU.add, None, reads=[tk[2]], writes=[tk[2]])
                    k.tt("dve", t[0][:], aref[:], aref[:], ALU.mult, reads=["c_aref"], writes=[tk[0]])
                    k.tt("dve", t[1][:], aimf[:], aimf[:], ALU.mult, reads=["c_aimf"], writes=[tk[1]])
                    k.tt("dve", t[0][:], t[0][:], t[1][:], ALU.add, reads=[tk[0], tk[1]], writes=[tk[0]])
                    k.op("dve", lambda g_: g_.reciprocal(out=t[0][:], in_=t[0][:]), reads=[tk[0]], writes=[tk[0]])
                    k.tt("dve", t[1][:], t[2][:], aref[:], ALU.mult, reads=[tk[2], "c_aref"], writes=[tk[1]])
                    k.tt("dve", t[4][:], t[3][:], aimf[:], ALU.mult, reads=[tk[3], "c_aimf"], writes=[tk[4]])
                    k.tt("dve", t[1][:], t[1][:], t[4][:], ALU.add, reads=[tk[1], tk[4]], writes=[tk[1]])
                    k.tt("dve", t[1][:], t[1][:], t[0][:], ALU.mult, reads=[tk[1], tk[0]], writes=[tk[1]])
                    k.tt("dve", t[4][:], t[3][:], aref[:], ALU.mult, reads=[tk[3], "c_aref"], writes=[tk[4]])
                    k.tt("dve", t[5][:], t[2][:], aimf[:], ALU.mult, reads=[tk[2], "c_aimf"], writes=[tk[5]])
                    k.tt("dve", t[4][:], t[4][:], t[5][:], ALU.subtract, reads=[tk[4], tk[5]], writes=[tk[4]])
                    k.tt("dve", t[4][:], t[4][:], t[0][:], ALU.mult, reads=[tk[4], tk[0]], writes=[tk[4]])
                    sg_bc = mk_ap(sgnf, 0, [[0, 16], [1, 128]])

                    def v3(tt_):
                        return mk_ap(tt_, 0, [[128, 16], [1, 128]])
                    k.tt("dve", v3(t[5]), v3(t[4]), sg_bc, ALU.mult, reads=[tk[4], "sgnf"], writes=[tk[5]])
                    k.tt("dve", t[5][:], t[5][:], bst_[:], ALU.mult, reads=[tk[5], "c_bst"], writes=[tk[5]])
                    k.tt("dve", t[2][:], t[1][:], brt[:], ALU.mult, reads=[tk[1], "c_brt"], writes=[tk[2]])
                    k.tt("dve", B1b[:], t[2][:], t[5][:], ALU.add, reads=[tk[2], tk[5]], writes=["B1b"])
                    k.tt("dve", v3(t[2]), v3(t[1]), sg_bc, ALU.mult, reads=[tk[1], "sgnf"], writes=[tk[2]])
                    k.tt("dve", t[2][:], t[2][:], bst_[:], ALU.mult, reads=[tk[2], "c_bst"], writes=[tk[2]])
                    k.tt("dve", t[5][:], t[4][:], brt[:], ALU.mult, reads=[tk[4], "c_brt"], writes=[tk[5]])
                    k.tt("dve", B2b[:], t[2][:], t[5][:], ALU.subtract, reads=[tk[2], tk[5]], writes=["B2b"])
                    k.ts("dve", rcb[:], T["l1"][:], sgn[:, 0:1], None, ALU.mult, None, reads=["c_l1", "sgn"], writes=["rcb"])
                    k.ts("dve", rc2b[:, 0:1024], T["l2"][:, 0:1024], -1.0, None, ALU.mult, None, reads=["c_l2"], writes=["rc2b"])
                    k.cp("dve", rc2b[:, 1024:2048], T["l2"][:, 1024:2048], reads=["c_l2"], writes=["rc2b"])
                    k.barrier()

                gT = k.sb(pc, "gT", [128, 8 * NOWN], BF16)
                tidx = k.sb(pc, "tidx", [128, TSEQ], F32)
                k.dma("sp", tidx[:], tidx_in[:, :], writes=["tidx"])
                pcm_cm = contextlib.ExitStack()
                with pcm_cm as pcm:
                    uct = [k.sb(pcm, "uct%d" % i, [128, TSEQ], BF16) for i in range(2)]
                    lb1s = [k.sb(pcm, "lb1_%d" % i, [128, 2048], BF16) for i in range(2)]
                    lb2s = [k.sb(pcm, "lb2_%d" % i, [128, 2048], BF16) for i in range(2)]
                    lrcs = [k.sb(pcm, "lrc_%d" % i, [128, 2048], BF16) for i in range(2)]
                    lrc2s = [k.sb(pcm, "lrc2_%d" % i, [128, 2048], BF16) for i in range(2)]
                    NR = 4

                    def mkring(name, dt_, depth):
                        return [k.sb(pcm, "%s%d" % (name, i), [128, 512], dt_) for i in range(depth)]
                    r_a1, r_a2 = mkring("a1_", F32, NR), mkring("a2_", F32, NR)
                    r_ns, r_nc = mkring("ns_", F32, NR), mkring("nc_", F32, NR)
                    r_tp, r_tm, r_W = mkring("tp_", F32, 2), mkring("tm_", F32, 2), mkring("W_", F32, 2)
                    r_Ao, r_Bo = mkring("Ao_", BF16, 2), mkring("Bo_", BF16, 2)
                    ytmp = k.sb(pcm, "ytmp", [128, NOWN], F32)
                    py = k.ps(pcm, "py", [128, NOWN], F32)
                    pb = [k.ps(pcm, "pb%d" % i, [128, 512], F32) for i in range(2)]
                    pb2 = [k.ps(pcm, "pb2%d" % i, [128, 512], F32) for i in range(2)]
                    pykeys = ["py%d" % j for j in range(4)]
                    segsA = [(4096, 256, 0, False)] + [(j * 512, 512, 256 + j * 512, True) for j in range(4)]
                    segsB = [(4096, 256, 4096, False)] + [(j * 512, 512, j * 512, j < 4) for j in range(7, -1, -1)]

                    def prep_ct(ct):
                        par = ct % 2
                        k.dma("sp", uct[par][:], uT_d[ct, :, :], reads=["uT_d"], writes=["uct%d" % par])
                        lb1, lb2, lrc, lrc2 = lb1s[par], lb2s[par], lrcs[par], lrc2s[par]
                        for slot in range(2):
                            blk = (ct * 2 + slot) * 128
                            k.tt("dve", mk_ap(lb1, slot * 1024, [[128, 8], [1, 128]]), mk_ap(B1b, blk, [[0, 8], [1, 128]]),
                                 mk_ap(gmask, 0, [[1, 8], [0, 128]]), ALU.mult, reads=["B1b", "gmask"],
                                 writes=["lb1_%d" % par])
                            gm = gmask if slot == 0 else gmaskn
                            k.tt("dve", mk_ap(lb2, slot * 1024, [[128, 8], [1, 128]]), mk_ap(B2b, blk, [[0, 8], [1, 128]]),
                                 mk_ap(gm, 0, [[1, 8], [0, 128]]), ALU.mult, reads=["B2b", "gmask", "gmaskn"],
                                 writes=["lb2_%d" % par])
                            k.memset("dve", lrc[:, slot * 1024:(slot + 1) * 1024], 0.0, writes=["lrc_%d" % par])
                            k.cp("dve", mk_ap(lrc, slot * 1024, [[144, 8], [1, 16]]),
                                 mk_ap(rcb, (slot * 64 + ct * 8) * 16, [[16, 8], [1, 16]]), reads=["rcb"],
                                 writes=["lrc_%d" % par])
                            k.memset("dve", lrc2[:, slot * 1024:(slot + 1) * 1024], 0.0, writes=["lrc2_%d" % par])
                            k.cp("dve", mk_ap(lrc2, slot * 1024, [[144, 8], [1, 16]]),
                                 mk_ap(rc2b, (slot * 64 + ct * 8) * 16, [[16, 8], [1, 16]]), reads=["rc2b"],
                                 writes=["lrc2_%d" % par])

                    segl = []
                    for ct in range(8):
                        for slot, segs in ((0, segsA), (1, segsB)):
                            for gi in range(8):
                                for si, (c0, n, tabs0, own) in enumerate(segs):
                                    segl.append(dict(ct=ct, slot=slot, gi=gi, c0=c0, n=n, tabs0=tabs0, own=own,
                                                     first=(si == 0), col=slot * 64 + ct * 8 + gi,
                                                     wo=slot * 1024 + gi * 128, q=len(segl), last_of_ct=False))
                        segl[-1]["last_of_ct"] = True
                    chain = {}

                    def kk_(nm, sg):
                        return "%s%d" % (nm, sg["q"] % NR)

                    def k2_(nm, sg):
                        return "%s%d" % (nm, sg["q"] % 2)

                    def stA(sg):
                        i, n = sg["q"] % NR, sg["n"]
                        a1, a2 = r_a1[i], r_a2[i]
                        k.act(a1[:, 0:n], tidx[:, sg["tabs0"]:sg["tabs0"] + n], AF.Copy, reads=["tidx", "w2p"],
                              writes=[kk_("a1_", sg)], scale=w2p[:, sg["col"]:sg["col"] + 1])
                        k.act(a2[:, 0:n], a1[:, 0:n], AF.Identity, reads=[kk_("a1_", sg), "magp"],
                              writes=[kk_("a2_", sg)], bias=magp[:, 0:1])
                        k.act(a2[:, 0:n], a2[:, 0:n], AF.Identity, reads=[kk_("a2_", sg), "magn"],
                              writes=[kk_("a2_", sg)], bias=magn[:, 0:1])
                        k.tt("pool", a1[:, 0:n], a1[:, 0:n], a2[:, 0:n], ALU.subtract,
                             reads=[kk_("a1_", sg), kk_("a2_", sg)], writes=[kk_("a1_", sg)])

                    def stB(sg):
                        i, n = sg["q"] % NR, sg["n"]
                        a1, a2, ns, ncs = r_a1[i], r_a2[i], r_ns[i], r_nc[i]
                        k.act(ns[:, 0:n], a1[:, 0:n], AF.Sin, reads=[kk_("a1_", sg)], writes=[kk_("ns_", sg)],
                              scale=TWO_PI_S)
                        k.act(a2[:, 0:n], a1[:, 0:n], AF.Abs, reads=[kk_("a1_", sg)], writes=[kk_("a2_", sg)])
                        k.act(ncs[:, 0:n], a2[:, 0:n], AF.Sin, reads=[kk_("a2_", sg), "halfpi"],
                              writes=[kk_("nc_", sg)], scale=-TWO_PI, bias=halfpi[:, 0:1])

                    def stP(sg):
                        ip, n, c0, wo, par = sg["q"] % 2, sg["n"], sg["c0"], sg["wo"], sg["ct"] % 2
                        ub, ubk = uct[par], "uct%d" % par
                        k.mm(pb[ip][:, 0:n], lb1s[par][:, wo:wo + 128], ub[:, c0:c0 + n], True, True,
                             reads=["lb1_%d" % par, ubk], writes=["pb%d" % ip], sig=False)
                        k.mm(pb2[ip][:, 0:n], lb2s[par][:, wo:wo + 128], ub[:, c0:c0 + n], True, True,
                             reads=["lb2_%d" % par, ubk], writes=["pb2%d" % ip], sig=True)

                    def stC(sg):
                        i, ip, n, c0, wo = sg["q"] % NR, sg["q"] % 2, sg["n"], sg["c0"], sg["wo"]
                        slot, gi, col, ct, par = sg["slot"], sg["gi"], sg["col"], sg["ct"], sg["ct"] % 2
                        desc = slot == 1
                        ns, ncs = r_ns[i], r_nc[i]
                        tp, tm, W = r_tp[ip], r_tm[ip], r_W[ip]
                        k.tt("dve", tp[:, 0:n], ncs[:, 0:n], pb[ip][:, 0:n], ALU.mult,
                             reads=[kk_("nc_", sg), "pb%d" % ip], writes=[k2_("tp_", sg)])
                        yield
                        k.tt("dve", tm[:, 0:n], ns[:, 0:n], pb2[ip][:, 0:n], ALU.mult,
                             reads=[kk_("ns_", sg), "pb2%d" % ip], writes=[k2_("tm_", sg)])

                    def stC2(sg):
                        i, ip, n, c0, wo = sg["q"] % NR, sg["q"] % 2, sg["n"], sg["c0"], sg["wo"]
                        slot, gi, col, ct, par = sg["slot"], sg["gi"], sg["col"], sg["ct"], sg["ct"] % 2
                        desc = slot == 1
                        ns, ncs = r_ns[i], r_nc[i]
                        tp, tm, W = r_tp[ip], r_tm[ip], r_W[ip]
                        k.tt("dve", tp[:, 0:n], tp[:, 0:n], tm[:, 0:n], ALU.subtract,
                             reads=[k2_("tp_", sg), k2_("tm_", sg)], writes=[k2_("tp_", sg)])
                        yield
                        if not desc:
                            d1 = tp[:, 0:n]
                            o_ = W[:, 0:n]
                        else:
                            d1 = mk_ap(tp, n - 1, [[-1, n]])
                            o_ = mk_ap(W, n - 1, [[-1, n]])
                        d0 = mk_ap(Rp, col, [[0, n]])
                        if sg["first"]:
                            init, rds = 0.0, [k2_("tp_", sg), "Rp"]
                        else:
                            carry, prevWk = chain[(ct, slot, gi)]
                            init, rds = carry, [k2_("tp_", sg), "Rp", prevWk]
                        k.op("dve", lambda g_: g_.tensor_tensor_scan(out=o_, data0=d0, data1=d1, initial=init,
                                                                      op0=ALU.mult, op1=ALU.add),
                             reads=rds, writes=[k2_("W_", sg)])
                        chain[(ct, slot, gi)] = ((W[:, n - 1:n] if not desc else W[:, 0:1]), k2_("W_", sg))
                        if sg["own"]:
                            Ao, Bo = r_Ao[ip], r_Bo[ip]
                            k.tt("pool", Ao[:, 0:n], ncs[:, 0:n], W[:, 0:n], ALU.mult,
                                 reads=[kk_("nc_", sg), k2_("W_", sg)], writes=[k2_("Ao_", sg)])
                            k.tt("pool", Bo[:, 0:n], ns[:, 0:n], W[:, 0:n], ALU.mult,
                                 reads=[kk_("ns_", sg), k2_("W_", sg)], writes=[k2_("Bo_", sg)])
                            first = (slot == 0 and gi == 0)
                            lastmm = (slot == 1 and gi == 7)
                            bank = c0 // 512
                            k.mm(py[:, c0:c0 + n], lrcs[par][:, wo:wo + 128], Ao[:, 0:n], first, False,
                                 reads=["lrc_%d" % par, k2_("Ao_", sg)], writes=[pykeys[bank]], sig=False)
                            k.mm(py[:, c0:c0 + n], lrc2s[par][:, wo:wo + 128], Bo[:, 0:n], False, lastmm,
                                 reads=["lrc2_%d" % par, k2_("Bo_", sg)], writes=[pykeys[bank]], sig=True)
                        if sg["last_of_ct"]:
                            k.stt(ytmp[:], uct[par][:, 0:NOWN], dsk[:, ct:ct + 1], py[:, :], ALU.mult, ALU.add,
                                  reads=["uct%d" % par, "dsk"] + pykeys, writes=["ytmp"])
                            k.act(gT[:, ct * NOWN:(ct + 1) * NOWN], ytmp[:], AF.Gelu_apprx_tanh, reads=["ytmp"],
                                  writes=["gT"])
                            if ct + 2 < 8:
                                prep_ct(ct + 2)

                    prep_ct(0)
                    prep_ct(1)
                    L_ = len(segl)
                    def _run(gens):
                        gens = [g for g in gens if g is not None]
                        while gens:
                            for g in list(gens):
                                try:
                                    next(g)
                                except StopIteration:
                                    gens.remove(g)

                    for it in range(L_ + 4):
                        if it % 14 == 0 and cv_jobs:
                            cj = cv_jobs.pop(0)
                            k.dma("cv", cj[0], cj[1], writes=[cj[2]])
                        if 0 <= it - 2 < L_:
                            stP(segl[it - 2])
                        _run([stC(segl[it - 3]) if 0 <= it - 3 < L_ else None,
                              stC2(segl[it - 4]) if 0 <= it - 4 < L_ else None])
                        if 0 <= it - 2 < L_:
                            stB(segl[it - 2])
                        if it < L_:
                            stA(segl[it])
                    k.barrier()
                while cv_jobs:
                    cj = cv_jobs.pop(0)
                    k.dma("cv", cj[0], cj[1], writes=[cj[2]])
                wglu = k.sb(pc, "wglu", [128, 8 * 1024], BF16)
                gst = [k.sb(pc, "gst%d" % i, [128, 1024], F32) for i in range(2)]
                cntg = [0]
                for ci in range(8):
                    load_cast(pc, wglu[:, ci * 1024:(ci + 1) * 1024], "wglu", w_glu[ci * 128:(ci + 1) * 128, :], 1024,
                              gst, ["gst0", "gst1"], cntg)
                k.barrier()
                sgr = [k.sb(pc, "sg_%d" % i, [128, 512], F32) for i in range(2)]
                pb = [k.ps(pc, "pbg%d" % i, [128, 512], F32) for i in range(2)]
                ng = 0
                for cot in range(8):
                    for tb in range(4):
                        i = ng % 2
                        ng += 1
                        pg, pgk = pb[i], "pbg%d" % i
                        for ci in range(8):
                            k.mm(pg[:, :], wglu[:, ci * 1024 + cot * 128:ci * 1024 + (cot + 1) * 128],
                                 gT[:, ci * NOWN + tb * 512:ci * NOWN + (tb + 1) * 512], ci == 0, ci == 7,
                                 reads=["wglu", "gT"], writes=[pgk])
                        k.act(sgr[i][:], pg[:], AF.Sigmoid, reads=[pgk, "bglu"], writes=["sg_%d" % i],
                              bias=bglu[:, cot:cot + 1])
                        k.tt("dve", yaT[:, cot * NOWN + tb * 512:cot * NOWN + (tb + 1) * 512],
                             gT[:, cot * NOWN + tb * 512:cot * NOWN + (tb + 1) * 512], sgr[i][:], ALU.mult,
                             reads=["gT", "sg_%d" % i], writes=["yaT"])
                if dbg:
                    k.dma("sp", dbg_g[:, :, :].rearrange("c p t -> p c t"), mk_ap(gT, 0, [[NOWN, 8], [1, NOWN]]),
                          reads=["gT"])
                    k.dma("sp", dbg_ya[:, :, :].rearrange("c p t -> p c t"), mk_ap(yaT, 0, [[NOWN, 8], [1, NOWN]]),
                          reads=["yaT"])
                k.barrier(include_cv=True)

        if last >= 3:
            with contextlib.ExitStack() as pd:
                g1bc = k.sb(pd, "g1bc", [128, D], F32)
                k.dma("sp", g1bc[:], rows_d[0, :, :], reads=["rows_d"], writes=["g1bc"])
                hTd = [k.sb(pd, "hTd%d" % i, [128, 16 * 512], BF16) for i in range(2)]
                gateT = k.sb(pd, "gateT", [128, 32 * 512], BF16)
                ybb = k.sb(pd, "ybb", [128, 8 * 512], BF16)
                mT = k.sb(pd, "mT", [128, 16 * 512], BF16)
                xs4 = [k.sb(pd, "xs4_%d" % i, [128, D], F32) for i in range(4)]
                junk = k.sb(pd, "junk", [128, D], BF16)
                ss = k.sb(pd, "ss", [128, 1], F32)
                rstd = k.sb(pd, "rstd", [128, 1], F32)
                xn = k.sb(pd, "xn", [128, D], BF16)
                NW = 4
                wbf = [k.sb(pd, "wbf%d" % i, [128, 16 * 256], BF16) for i in range(NW)]
                t1 = k.sb(pd, "t1", [128, 512], F32)
                t2 = k.sb(pd, "t2", [128, 512], F32)
                pT = k.ps(pd, "pT", [128, D], BF16)
                pg = [k.ps(pd, "pg%d" % i, [128, 512], F32) for i in range(2)]
                pA = k.ps(pd, "pA", [128, 512], F32)
                pB = k.ps(pd, "pB", [128, 512], F32)
                pO = [k.ps(pd, "pO%d" % i, [128, 128], F32) for i in range(2)]
                nw = [0]
                wcache = {}

                def wunit(src16, col0, nk):
                    u_ = col0 // 256
                    ck = id(src16.tensor)
                    if wcache.get(ck, (None,))[0] != u_:
                        i = nw[0] % NW
                        nw[0] += 1
                        k.dma("sp", mk_ap(wbf[i], 0, [[256, nk], [1, 256]]),
                              src16[0:nk * 128, u_ * 256:(u_ + 1) * 256].rearrange("(kc p) c -> p kc c", p=128),
                              reads=["w16"], writes=["wbf%d" % i])
                        wcache[ck] = (u_, i)
                    i = wcache[ck][1]
                    off = col0 % 256
                    return (lambda kc: wbf[i][:, kc * 256 + off:kc * 256 + off + 128]), "wbf%d" % i

                npg = 0
                npo = 0
                for blk in range(4):
                    row0 = blk * 512
                    hT = hTd[blk % 2]
                    hkd = "hTd%d" % (blk % 2)
                    k.dma("sp", mk_ap(hT, 0, [[512, 16], [1, 512]]),
                          hT_d[:, :, row0:row0 + 512].rearrange("c p t -> p c t"), reads=["hT_d"], writes=[hkd])
                    for j in range(4):
                        k.dma("sp", xs4[j][:], xs[row0 + j * 128:row0 + (j + 1) * 128, :], writes=["xs4_%d" % j])
                    k.dma("sp", mk_ap(ybb, 0, [[512, 8], [1, 512]]),
                          ybT_d[:, :, row0:row0 + 512].rearrange("c p t -> p c t"), reads=["ybT_d"], writes=["ybb"])
                    for f in range(32):
                        wb, wk = wunit(w16g_d, f * 128, 16)
                        p_ = pg[npg % 2]
                        pk = "pg%d" % (npg % 2)
                        npg += 1
                        for kc in range(16):
                            k.mm(p_[:, :], wb(kc), hT[:, kc * 512:(kc + 1) * 512],
                                 kc == 0, kc == 15, reads=[wk, hkd], writes=[pk])
                        k.act(gateT[:, f * 512:(f + 1) * 512], p_[:, :], AF.Sigmoid, reads=[pk], writes=["gateT"])
                    for dt_ in range(16):
                        wb, wk = wunit(pa16_d, dt_ * 128, 8)
                        for ci in range(8):
                            k.mm(pA[:, :], wb(ci),
                                 yaT[:, ci * NOWN + row0:ci * NOWN + row0 + 512], ci == 0, ci == 7,
                                 reads=[wk, "yaT"], writes=["pA"])
                        wb, wk = wunit(pb16_d, dt_ * 128, 8)
                        for ci in range(8):
                            k.mm(pB[:, :], wb(ci), ybb[:, ci * 512:(ci + 1) * 512],
                                 ci == 0, ci == 7, reads=[wk, "ybb"], writes=["pB"])
                        k.tt("dve", t1[:], pA[:, :], gateT[:, dt_ * 512:(dt_ + 1) * 512], ALU.mult,
                             reads=["pA", "gateT"], writes=["t1"])
                        k.tt("dve", t2[:], pB[:, :], gateT[:, (16 + dt_) * 512:(17 + dt_) * 512], ALU.mult,
                             reads=["pB", "gateT"], writes=["t2"])
                        k.tt("dve", mT[:, dt_ * 512:(dt_ + 1) * 512], t1[:], t2[:], ALU.add,
                             reads=["t1", "t2"], writes=["mT"])
                    for dt_ in range(16):
                        wb, wk = wunit(wo16_d, dt_ * 128, 16)
                        for j in range(4):
                            po = pO[npo % 2]
                            pok = "pO%d" % (npo % 2)
                            npo += 1
                            for kc in range(16):
                                k.mm(po[:, :], mT[:, kc * 512 + j * 128:kc * 512 + (j + 1) * 128],
                                     wb(kc), kc == 0, kc == 15,
                                     reads=[wk, "mT"], writes=[pok])
                            k.tt("dve", t1[:, 0:128], po[:, :], g1bc[:, dt_ * 128:(dt_ + 1) * 128], ALU.mult,
                                 reads=[pok, "g1bc"], writes=["t1"])
                            k.tt("dve", xs4[j][:, dt_ * 128:(dt_ + 1) * 128], xs4[j][:, dt_ * 128:(dt_ + 1) * 128],
                                 t1[:, 0:128], ALU.add, reads=["t1", "xs4_%d" % j], writes=["xs4_%d" % j])
                    for j in range(4):
                        k.dma("sp", x1_d[row0 + j * 128:row0 + (j + 1) * 128, :], xs4[j][:],
                              reads=["xs4_%d" % j], writes=["x1_d"])
                k.barrier()
            pcd.close()

        if last >= 4:
            with contextlib.ExitStack() as pe_:
                NT = NOWN // 128
                eid_all = k.sb(pe_, "eid_all", [128, NT * 128], U32)
                gw_all = k.sb(pe_, "gw_all", [128, NT * 128], F32)
                ss = k.sb(pe_, "ss", [128, 1], F32)
                rstd = k.sb(pe_, "rstd", [128, 1], F32)

                def rms_rstd(src, srck, junk_t, junkk):
                    k.act(junk_t[:], src[:], AF.Square, reads=[srck], writes=[junkk, "ss"], accum_out=ss[:, 0:1])
                    k.act(rstd[:, 0:1], ss[:, 0:1], AF.Ln, reads=["ss", "epsc"], writes=["rstd"], scale=1.0 / D,
                          bias=epsc[:, 0:1])
                    k.act(rstd[:, 0:1], rstd[:, 0:1], AF.Exp, reads=["rstd"], writes=["rstd"], scale=-0.5)

                with contextlib.ExitStack() as pe1:
                    wqb = k.sb(pe1, "wqb", [128, 16 * 2048], BF16)
                    sh2bc = k.sb(pe1, "sh2bc", [128, D], F32)
                    wm2bc = k.sb(pe1, "wm2bc", [128, D], F32)
                    kTb = k.sb(pe1, "kTb", [128, 256], BF16)
                    iota16 = k.sb(pe1, "iota16", [128, 16], F32)
                    k.dma("sp", sh2bc[:], rows_d[1, :, :], reads=["rows_d"], writes=["sh2bc"])
                    k.dma("sp", wm2bc[:], rows_d[2, :, :], reads=["rows_d"], writes=["wm2bc"])
                    k.dma("sp", iota16[:], iota16_in[:, :], writes=["iota16"])
                    stg = [k.sb(pe1, "stg%d" % i, [128, 2048], F32) for i in range(4)]
                    cnt = [0]
                    for kc in range(16):
                        load_cast(pe1, wqb[:, kc * 2048:(kc + 1) * 2048], "wqb", wq[kc * 128:(kc + 1) * 128, :], 2048,
                                  stg, ["stg%d" % i_ for i_ in range(4)], cnt)
                    load_cast(pe1, kTb[:, 0:128], "kTb", k1T[:, :], 128, stg, ["stg%d" % i_ for i_ in range(4)], cnt)
                    load_cast(pe1, kTb[:, 128:256], "kTb", k2T[:, :], 128, stg, ["stg%d" % i_ for i_ in range(4)], cnt)
                    k.barrier()
                    x1t = k.sb(pe1, "x1t", [128, D], F32)
                    h2 = k.sb(pe1, "h2", [128, D], F32)
                    h2b = k.sb(pe1, "h2b", [128, D], BF16)
                    h2T = k.sb(pe1, "h2T", [128, D], BF16)
                    qT = k.sb(pe1, "qT", [128, D], BF16)
                    s_r = [k.sb(pe1, "s_%d" % i, [128, D], F32) for i in range(2)]
                    s2 = [k.sb(pe1, "s2_%d" % i, [128, 256], F32) for i in range(2)]
                    V = k.sb(pe1, "V", [128, 256], F32)
                    I_ = k.sb(pe1, "I_", [128, 256], U32)
                    If = k.sb(pe1, "If", [128, 256], F32)
                    cand = k.sb(pe1, "cand", [128, D], F32)
                    OH = k.sb(pe1, "OH", [128, D], F32)
                    tops = k.sb(pe1, "tops", [128, 128], F32)
                    pos = k.sb(pe1, "pos", [128, 128], U32)
                    posf = k.sb(pe1, "posf", [128, 128], F32)
                    af = k.sb(pe1, "af", [128, 128], F32)
                    bf_ = k.sb(pe1, "bf_", [128, 128], F32)
                    ee = k.sb(pe1, "ee", [128, 128], F32)
                    Z = k.sb(pe1, "Z", [128, 8], F32)
                    I1s = k.sb(pe1, "I1s", [128, 128], F32)
                    I2s = k.sb(pe1, "I2s", [128, 128], F32)
                    eidf = k.sb(pe1, "eidf", [128, 128], F32)
                    pT2 = k.ps(pe1, "pT2", [128, D], BF16)
                    pq = k.ps(pe1, "pq", [128, D], F32)

                    def top16(src_ap, srck, vdst, idst, vk, ik, scratch, sk):
                        k.op("dve", lambda g_: g_.max(out=vdst[:, 0:8], in_=src_ap), reads=[srck], writes=[vk])
                        k.op("dve", lambda g_: g_.max_index(out=idst[:, 0:8], in_max=vdst[:, 0:8], in_values=src_ap),
                             reads=[srck, vk], writes=[ik])
                        k.op("dve", lambda g_: g_.match_replace(out=scratch, in_to_replace=vdst[:, 0:8], in_values=src_ap,
                                                                imm_value=NEG_BIG), reads=[srck, vk], writes=[sk])
                        k.op("dve", lambda g_: g_.max(out=vdst[:, 8:16], in_=scratch), reads=[sk], writes=[vk])
                        k.op("dve", lambda g_: g_.max_index(out=idst[:, 8:16], in_max=vdst[:, 8:16], in_values=scratch),
                             reads=[sk, vk], writes=[ik])

                    def top16_multi(items):
                        for (src, srck, vd, idt, vk, ik, scr, sk) in items:
                            k.op("dve", lambda g_: g_.max(out=vd[:, 0:8], in_=src), reads=[srck], writes=[vk])
                        for (src, srck, vd, idt, vk, ik, scr, sk) in items:
                            k.op("dve", lambda g_: g_.max_index(out=idt[:, 0:8], in_max=vd[:, 0:8], in_values=src),
                                 reads=[srck, vk], writes=[ik])
                        for (src, srck, vd, idt, vk, ik, scr, sk) in items:
                            k.op("dve", lambda g_: g_.match_replace(out=scr, in_to_replace=vd[:, 0:8], in_values=src,
                                                                    imm_value=NEG_BIG), reads=[srck, vk], writes=[sk])
                        for (src, srck, vd, idt, vk, ik, scr, sk) in items:
                            k.op("dve", lambda g_: g_.max(out=vd[:, 8:16], in_=scr), reads=[sk], writes=[vk + "b"])
                        for (src, srck, vd, idt, vk, ik, scr, sk) in items:
                            k.op("dve", lambda g_: g_.max_index(out=idt[:, 8:16], in_max=vd[:, 8:16], in_values=scr),
                                 reads=[sk, vk + "b"], writes=[ik + "b"])

                    def e1_stage1(tile_i):
                        r0 = tile_i * 128
                        s_ = s_r[tile_i % 2]
                        sk_ = "s_%d" % (tile_i % 2)
                        k.dma("sp", x1t[:], x1_d[r0:r0 + 128, :], reads=["x1_d"], writes=["x1t"])
                        rms_rstd(x1t, "x1t", h2b, "h2b")
                        k.act(h2[:], x1t[:], AF.Copy, reads=["x1t", "rstd"], writes=["h2"], scale=rstd[:, 0:1])
                        k.tt("pool", h2[:], h2[:], wm2bc[:], ALU.mult, reads=["h2", "wm2bc"], writes=["h2"])
                        k.tt("pool", h2[:], h2[:], sh2bc[:], ALU.add, reads=["h2", "sh2bc"], writes=["h2"])
                        k.act(h2b[:], h2[:], AF.Copy, reads=["h2"], writes=["h2b"])
                        k.dma("sp", h2b_d[r0:r0 + 128, :], h2b[:], reads=["h2b"], writes=["h2b_d"])
                        for kc in range(16):
                            k.tr(pT2[:, kc * 128:(kc + 1) * 128], h2b[:, kc * 128:(kc + 1) * 128], ident_b[:],
                                 reads=["h2b", "ident_b"], writes=["pT2"], sig=(kc == 15))
                        k.act(h2T[:], pT2[:], AF.Copy, reads=["pT2"], writes=["h2T"])
                        for f in range(16):
                            for kc in range(16):
                                k.mm(pq[:, f * 128:(f + 1) * 128], wqb[:, kc * 2048 + f * 128:kc * 2048 + (f + 1) * 128],
                                     h2T[:, kc * 128:(kc + 1) * 128], kc == 0, kc == 15, reads=["wqb", "h2T"], writes=["pq"],
                                     sig=(f == 15 and kc == 15))
                        k.act(qT[:], pq[:], AF.Copy, reads=["pq"], writes=["qT"])
                        for f in range(16):
                            k.mm(pq[:, f * 128:(f + 1) * 128], qT[:, f * 128:(f + 1) * 128],
                                 kTb[:, (f % 2) * 128:(f % 2 + 1) * 128], True, True, reads=["qT", "kTb"], writes=["pq"],
                                 sig=(f == 15))
                        k.act(s_[:], pq[:], AF.Copy, reads=["pq"], writes=[sk_])

                    def e1_stage2(tile_i):
                        eid = eid_all[:, tile_i * 128:(tile_i + 1) * 128]
                        gw = gw_all[:, tile_i * 128:(tile_i + 1) * 128]
                        s_ = s_r[tile_i % 2]
                        sk_ = "s_%d" % (tile_i % 2)
                        for f0 in range(0, 16, 2):
                            top16_multi([(s_[:, f * 128:(f + 1) * 128], sk_, V[:, f * 16:(f + 1) * 16],
                                          I_[:, f * 16:(f + 1) * 16], "V%d" % f, "I%d" % f, s2[f % 2][:, 0:128],
                                          "s2_%d" % (f % 2)) for f in (f0, f0 + 1)])
                        Vkeys = ["V%d" % f for f in range(16)] + ["V%db" % f for f in range(16)]
                        Ikeys = ["I%d" % f for f in range(16)] + ["I%db" % f for f in range(16)]
                        k.tt("dve", mk_ap(cand, 0, [[256, 8], [16, 16], [1, 16]]), mk_ap(V, 0, [[32, 8], [1, 16], [0, 16]]),
                             mk_ap(V, 16, [[32, 8], [0, 16], [1, 16]]), ALU.add, reads=Vkeys, writes=["cand"])
                        for h0 in range(0, 8, 2):
                            top16_multi([(cand[:, h * 256:(h + 1) * 256], "cand", tops[:, h * 16:(h + 1) * 16],
                                          pos[:, h * 16:(h + 1) * 16], "tp%d" % h, "ps%d" % h, s2[h % 2][:, 0:256],
                                          "s2_%d" % (h % 2)) for h in (h0, h0 + 1)])
                        Tkeys = ["tp%d" % h for h in range(8)] + ["tp%db" % h for h in range(8)]
                        Pkeys = ["ps%d" % h for h in range(8)] + ["ps%db" % h for h in range(8)]
                        k.tt("dve", mk_ap(ee, 0, [[16, 8], [1, 16]]), mk_ap(tops, 0, [[16, 8], [1, 16]]),
                             mk_ap(tops, 0, [[16, 8], [0, 16]]), ALU.subtract, reads=Tkeys, writes=["ee"])
                        k.act(ee[:], ee[:], AF.Exp, reads=["ee"], writes=["ee"])
                        k.op("dve", lambda g_: g_.tensor_reduce(out=Z[:], in_=mk_ap(ee, 0, [[16, 8], [1, 16]]),
                                                                axis=AX.X, op=ALU.add), reads=["ee"], writes=["Z"])
                        k.op("dve", lambda g_: g_.reciprocal(out=Z[:], in_=Z[:]), reads=["Z"], writes=["Z"])
                        k.tt("dve", mk_ap(gw, 0, [[16, 8], [1, 16]]), mk_ap(ee, 0, [[16, 8], [1, 16]]),
                             mk_ap(Z, 0, [[1, 8], [0, 16]]), ALU.mult, reads=["ee", "Z"], writes=["gw_all"])
                        k.cp("dve", posf[:], pos[:], reads=Pkeys, writes=["posf"])
                        k.cp("dve", If[:], I_[:], reads=Ikeys, writes=["If"])
                        k.ts("dve", af[:], posf[:], 1.0 / 16, -0.46875, ALU.mult, ALU.add, reads=["posf"], writes=["af"])
                        k.ts("dve", af[:], af[:], MAGIC, MAGIC, ALU.add, ALU.subtract, reads=["af"], writes=["af"])
                        k.stt(bf_[:], af[:], -16.0, posf[:], ALU.mult, ALU.add, reads=["af", "posf"], writes=["bf_"])
                        for (sel, src_off, dst, dk) in ((af, 0, I1s, "I1s"), (bf_, 16, I2s, "I2s")):
                            selk = "af" if sel is af else "bf_"
                            k.tt("dve", mk_ap(OH, 0, [[256, 8], [16, 16], [1, 16]]),
                                 mk_ap(sel, 0, [[16, 8], [1, 16], [0, 16]]),
                                 mk_ap(iota16, 0, [[0, 8], [0, 16], [1, 16]]), ALU.is_equal, reads=[selk, "iota16"],
                                 writes=["OH"])
                            k.tt("dve", mk_ap(OH, 0, [[256, 8], [16, 16], [1, 16]]),
                                 mk_ap(OH, 0, [[256, 8], [16, 16], [1, 16]]),
                                 mk_ap(If, src_off, [[32, 8], [0, 16], [1, 16]]), ALU.mult, reads=["OH", "If"], writes=["OH"])
                            k.op("dve", lambda g_: g_.tensor_reduce(out=dst[:], in_=mk_ap(OH, 0, [[16, 128], [1, 16]]),
                                                                    axis=AX.X, op=ALU.add), reads=["OH"], writes=[dk])
                        k.stt(eidf[:], I1s[:], 128.0, I2s[:], ALU.mult, ALU.add, reads=["I1s", "I2s"], writes=["eidf"])
                        k.cp("dve", eid, eidf[:], reads=["eidf"], writes=["eid_all"])

                    e1_stage1(0)
                    for tile_i in range(NT):
                        if tile_i + 1 < NT:
                            e1_stage1(tile_i + 1)
                        e1_stage2(tile_i)
                    k.barrier()

                with contextlib.ExitStack() as pe3:
                    g2bc = k.sb(pe3, "g2bc", [128, D], F32)
                    fwbc = k.sb(pe3, "fwbc", [128, D], F32)
                    k.dma("sp", g2bc[:], rows_d[3, :, :], reads=["rows_d"], writes=["g2bc"])
                    k.dma("sp", fwbc[:], fw_bc[:, :], writes=["fwbc"])
                    x1r = [k.sb(pe3, "x1r%d" % i, [128, D], F32) for i in range(2)]
                    h2r = [k.sb(pe3, "h2r%d" % i, [128, D], BF16) for i in range(2)]
                    junkb = k.sb(pe3, "junkb", [128, D], BF16)
                    junk2 = k.sb(pe3, "junk2", [128, D], BF16)
                    dots = k.sb(pe3, "dots", [128, 128], F32)
                    gel = k.sb(pe3, "gel", [128, 128], F32)
                    ot = k.sb(pe3, "ot", [128, D], F32)
                    NBUV, NDG = 8, 4
                    uvr = [k.sb(pe3, "uv%d" % i, [128, 2 * D], BF16) for i in range(NBUV)]
                    dgr = [k.sb(pe3, "dg%d" % i, [128, 128], BF16) for i in range(NDG)]
                    paccs = [k.ps(pe3, "pacc%d" % i, [128, D], F32) for i in range(2)]
                    cn = {"uv": 0, "d": 0}
                    for tile_i in range(NT):
                        r0 = tile_i * 128
                        par = tile_i % 2
                        x1t, x1k, h2t, h2k = x1r[par], "x1r%d" % par, h2r[par], "h2r%d" % par
                        pacc = paccs[par]
                        pkeys = ["pacc%d_%d" % (par, j) for j in range(4)]
                        k.dma("sp", x1t[:], x1_d[r0:r0 + 128, :], reads=["x1_d"], writes=[x1k])
                        k.dma("sp", h2t[:], h2b_d[r0:r0 + 128, :], reads=["h2b_d"], writes=[h2k])
                        pend = None
                        for slot in range(128):
                            uv, uvk = uvr[cn["uv"] % NBUV], "uv%d" % (cn["uv"] % NBUV)
                            cn["uv"] += 1
                            gcol = tile_i * 128 + slot
                            k.dma("pool", uv[:], uv16_d[:, :], reads=["eid_all", "u16_d", "v16_d"], writes=[uvk],
                                  idx=eid_all[:, gcol:gcol + 1])
                            k.stt(junkb[:], uv[:, 0:D], 1.0, h2t[:], ALU.mult, ALU.mult, reads=[uvk, h2k],
                                  writes=["junkb", "dots%d" % slot], accum_out=dots[:, slot:slot + 1])
                            k.act(gel[:, slot:slot + 1], dots[:, slot:slot + 1], AF.Gelu_apprx_tanh,
                                  reads=["dots%d" % slot], writes=["gel%d" % slot])
                            cur = (slot, uv, uvk, gcol)
                            for (sl_, uv_, uvk_, gcol_) in ([pend] if pend else []) + ([cur] if slot == 127 else []):
                                dg, dgk = dgr[cn["d"] % NDG], "dg%d" % (cn["d"] % NDG)
                                cn["d"] += 1
                                k.ts("dve", dg[:], ident_f[:], gel[:, sl_:sl_ + 1], gw_all[:, gcol_:gcol_ + 1],
                                     ALU.mult, ALU.mult, reads=["ident_f", "gel%d" % sl_, "gw_all"], writes=[dgk])
                                for nb in range(4):
                                    k.mm(pacc[:, nb * 512:(nb + 1) * 512], dg[:], uv_[:, D + nb * 512:D + (nb + 1) * 512],
                                         sl_ == 0, sl_ == 127, reads=[dgk, uvk_], writes=[pkeys[nb]], sig=(nb == 3))
                            pend = cur
                        k.tt("dve", ot[:], pacc[:], g2bc[:], ALU.mult, reads=pkeys + ["g2bc"], writes=["ot"])
                        k.tt("dve", ot[:], ot[:], x1t[:], ALU.add, reads=["ot", x1k], writes=["ot"])
                        rms_rstd(ot, "ot", junk2, "junk2")
                        k.stt(ot[:], ot[:], rstd[:, 0:1], fwbc[:], ALU.mult, ALU.mult, reads=["ot", "rstd", "fwbc"],
                              writes=["ot"])
                        k.dma("sp", y_out[r0:r0 + 128, :], ot[:], reads=["ot"], writes=["y_out"])
                    k.barrier()

        k.final_wait()
    return nc


def _pool_mats(flip):
    wins = (2, 4, 8, 16)
    out = np.zeros((128, 4, 128), np.float32)
    for g, w in enumerate(wins):
        P = np.zeros((64, 64), np.float32)
        for pos in range(64):
            lo = min(max(pos - w // 2, 0), 63)
            hi = min(max(pos + w // 2 - 1, 0), 63)
            P[lo:hi + 1, pos] += 1.0 / (hi - lo + 1)
            P[pos, pos] -= 1.0
        if flip:
            P = P[::-1, ::-1]
        out[0:64, g, 0:64] = P
        out[64:128, g, 64:128] = P
    return out.reshape(128, 512)


def _fm(v, n):
    return np.ascontiguousarray(np.asarray(v, np.float32).reshape(n, 128).T)


def prep_core(inp, b, half, last):
    flip = half == 1
    x = inp["x"][b]
    ctx = inp["ctx"][b]
    if flip:
        x = x[::-1]
        ctx = ctx[::-1]
    m = {}
    m["xs"] = np.ascontiguousarray(np.concatenate([x, ctx], axis=0), dtype=np.float32)
    m["cT"] = np.concatenate([_fm(inp["c"][b], 16), _fm(inp["c_ctx"], 16)], axis=1)
    m["w_mod"] = inp["w_mod"][0]
    m["bmod_bc"] = np.ascontiguousarray(np.broadcast_to(inp["b_mod"][0][None, :], (128, 6 * D)))
    m["n1T"] = _fm(inp["norm1_w"][0], 16)
    m["n2_bc"] = np.ascontiguousarray(np.broadcast_to(inp["norm2_w"][0][None, :], (128, D)))
    m["fw_bc"] = np.ascontiguousarray(np.broadcast_to(inp["final_w"][None, :], (128, D)))
    m["w_in"] = inp["w_in"][0]
    m["ident"] = np.eye(128, dtype=np.float32)
    m["pm2"] = _pool_mats(flip)
    m["poolw"] = np.ascontiguousarray(inp["pool_w"][0].reshape(1024, 256))
    m["pscT"] = _fm(inp["pool_scale"][0], 8)
    e0 = np.zeros((128, 1), np.float32)
    e0[0, 0] = 1.0
    m["e0"] = e0

    if last >= 2:
        dsel = [1, 0] if flip else [0, 1]
        a_re = inp["s5_a_re"][0]; a_im = inp["s5_a_im"][0]
        ldt = np.broadcast_to(inp["s5_log_dt"][0][:, :, None], (2, 64, 64))

        def part_layout(a):
            arr = np.stack([a[dsel[0]], a[dsel[1]]], 0)
            t_ = arr.transpose(2, 0, 1).reshape(64, 128)
            return np.ascontiguousarray(np.concatenate([t_, t_], 0), dtype=np.float32)

        def free_layout(a):
            arr = np.stack([a[dsel[0]], a[dsel[1]]], 0).reshape(2, 8, 8, 64)
            arr = arr.transpose(2, 1, 0, 3)
            arr = np.broadcast_to(arr[:, None, :, :, None, :], (8, 16, 8, 2, 2, 64))
            return np.ascontiguousarray(arr.reshape(128, 2048), dtype=np.float32)

        def b_layout(b0, b1):
            out = np.zeros((8, 16, 8, 2, 2, 64), np.float32)
            for slot in range(2):
                d = dsel[slot]
                for ri, arr in ((0, b0), (1, b1)):
                    a = arr[d].reshape(8, 8, 64, 16)
                    out[:, :, :, slot, ri, :] = a.transpose(1, 3, 0, 2)
            return out.reshape(128, 2048)

        def c_layout(c0, c1):
            out = np.zeros((2, 64, 2, 64, 16), np.float32)
            for slot in range(2):
                d = dsel[slot]
                out[0, :, slot] = c0[d].transpose(2, 0, 1)
                out[1, :, slot] = c1[d].transpose(2, 0, 1)
            return out.reshape(128, 2048)

        m["tidx"] = np.ascontiguousarray(np.broadcast_to(np.arange(TSEQ, dtype=np.float32)[None, :], (128, TSEQ)))
        m["s5_arep"] = part_layout(a_re); m["s5_aimp"] = part_layout(a_im); m["s5_ldtp"] = part_layout(ldt)
        m["s5_aref"] = free_layout(a_re); m["s5_aimf"] = free_layout(a_im); m["s5_ldtf"] = free_layout(ldt)
        bre = inp["s5_b_re"][0]; bim = inp["s5_b_im"][0]
        m["s5_brt"] = b_layout(bre, bim); m["s5_bst"] = b_layout(bim, bre)
        cre = inp["s5_c_re"][0]; cim = inp["s5_c_im"][0]
        m["s5_l1"] = c_layout(cre, cim); m["s5_l2"] = c_layout(cim, cre)
        sgn = np.zeros((128, 132), np.float32)
        sgn[:64, 0] = 1.0; sgn[64:, 0] = -1.0; sgn[:, 1] = -1.0
        sgn[:, 4:68] = -1.0; sgn[:, 68:132] = 1.0
        m["sgn"] = sgn
        gm = np.zeros((128, 8), np.float32)
        for gi in range(8):
            gm[gi * 16:(gi + 1) * 16, gi] = 1.0
        m["gmask"] = gm
        m["dskT"] = _fm(inp["s5_d"][0], 8)
        m["bgluT"] = _fm(inp["b_glu"][0], 8)
        m["w_glu"] = inp["w_glu"][0]
    if last >= 3:
        m["proj_a"] = inp["proj_a"][0]
        m["proj_b"] = inp["proj_b"][0]
        m["w_out"] = inp["w_out"][0]
    if last >= 4:
        m["wq"] = inp["peer_wq"][0]
        m["k1T"] = np.ascontiguousarray(inp["peer_k1"][0].T)
        m["k2T"] = np.ascontiguousarray(inp["peer_k2"][0].T)
        m["peer_u"] = inp["peer_u"][0]
        m["peer_v"] = inp["peer_v"][0]
        m["iota16"] = np.ascontiguousarray(np.broadcast_to(np.arange(16, dtype=np.float32)[None, :], (128, 16)))
    return m


_PROG = {}


def kernel(**inputs):
    inp = {k_: np.asarray(v) for k_, v in inputs.items()}
    if "nc" not in _PROG:
        _PROG["nc"] = build_program(stop_after="E", dbg=False)
    nc = _PROG["nc"]
    in_maps = []
    for core in range(8):
        b, half = core // 2, core % 2
        in_maps.append(prep_core(inp, b, half, 4))
    res = run_bass_kernel_spmd(nc, in_maps, core_ids=list(range(8)))
    out = np.empty((4, SEQ, D), np.float32)
    for core in range(8):
        b, half = core // 2, core % 2
        y = np.asarray(res.results[core]["y"], dtype=np.float32)
        if half == 0:
            out[b, 0:NOWN] = y
        else:
            out[b, NOWN:SEQ] = y[::-1]
    return out
```

```python
import contextlib
import math
import numpy as np
import concourse.bass as bass
import concourse.mybir as mybir
from concourse.bass_utils import run_bass_kernel_spmd

F32 = mybir.dt.float32
BF16 = mybir.dt.bfloat16
U32 = mybir.dt.uint32
ALU = mybir.AluOpType
AF = mybir.ActivationFunctionType
AX = mybir.AxisListType
AP = bass.AP

D = 2048
SEQ = 4096
CTX = 256
NOWN = 2048
TSEQ = SEQ + CTX
EPS = 1e-6
TWO_PI = 2.0 * math.pi
TWO_PI_S = 6.28318
NEG_BIG = -1.0e30
MAGIC = 12582912.0
INV_2PI = 1.0 / (2.0 * math.pi)


def ap_of(t):
    return t[:] if not isinstance(t, AP) else t


def mk_ap(base, offset_elems, dims):
    b = ap_of(base)
    return AP(b.tensor, b.offset + offset_elems, [list(b.ap[0])] + [list(d) for d in dims])


class K:
    def __init__(self, nc, es):
        self.nc = nc
        self.es = es
        self.E = {"pe": nc.tensor, "dve": nc.vector, "act": nc.scalar, "pool": nc.gpsimd, "sp": nc.sync}
        self.tok = {}
        for e in ("pe", "dve", "act", "pool"):
            self.tok[e] = dict(sem=es.enter_context(nc.semaphore("s_" + e)), cnt=0, step=1)
        self.dq = {}
        self.qeng = {"sp": "sp", "pool": "pool", "act": "act", "cv": "pool"}
        self.keep = set()
        for q, n in (("sp", 16), ("pool", 8), ("act", 4), ("cv", 4)):
            names = []
            for i in range(n):
                nm = "d_%s%d" % (q, i)
                self.tok[nm] = dict(sem=es.enter_context(nc.semaphore(nm)), cnt=0, step=16)
                names.append(nm)
            self.dq[q] = [names, 0]
        self.waited = {e: {} for e in self.E}
        self.lastw = {}
        self.reads = {}
        self.ninst = 0

    def sb(self, es, name, shape, dt):
        self.ninst += 0
        self._uid = getattr(self, "_uid", 0) + 1
        return es.enter_context(self.nc.sbuf_tensor("sb%d_%s" % (self._uid, name), list(shape), dt))

    def ps(self, es, name, shape, dt=F32):
        self._uid = getattr(self, "_uid", 0) + 1
        return es.enter_context(self.nc.psum_tensor("ps%d_%s" % (self._uid, name), list(shape), dt))

    def _deps(self, reads, writes):
        deps = {}

        def add(tv):
            if tv is None:
                return
            t, v = tv
            if deps.get(t, 0) < v:
                deps[t] = v

        for k in reads:
            add(self.lastw.get(k))
        for k in writes:
            add(self.lastw.get(k))
            for t, v in self.reads.get(k, {}).items():
                add((t, v))
        return deps

    def _wait(self, e, deps):
        for t, v in deps.items():
            if t == e and e == "pe":
                continue
            if self.waited[e].get(t, 0) >= v:
                continue
            tk = self.tok[t]
            self.E[e].wait_ge(tk["sem"], v * tk["step"])
            self.waited[e][t] = v

    def _record(self, t, v, reads, writes):
        for k in reads:
            r = self.reads.setdefault(k, {})
            if r.get(t, 0) < v:
                r[t] = v
        for k in writes:
            self.lastw[k] = (t, v)
            self.reads[k] = {}

    def op(self, e, fn, reads=(), writes=(), sig=True):
        deps = self._deps(reads, writes)
        self._wait(e, deps)
        inst = fn(self.E[e])
        tk = self.tok[e]
        if sig:
            inst.then_inc(tk["sem"], 1)
            tk["cnt"] += 1
            v = tk["cnt"]
        else:
            v = tk["cnt"] + 1
        self._record(e, v, reads, writes)
        self.ninst += 1
        return inst

    def dma(self, q, out, in_, reads=(), writes=(), idx=None):
        names, rr = self.dq[q]
        nm = names[rr % len(names)]
        self.dq[q][1] += 1
        tk = self.tok[nm]
        deps = self._deps(reads, writes)
        if tk["cnt"] > 0 and deps.get(nm, 0) < tk["cnt"]:
            deps[nm] = tk["cnt"]
        qe = self.qeng[q]
        self._wait(qe, deps)
        if idx is None:
            inst = self.E[qe].dma_start(out=out, in_=in_)
        else:
            inst = self.E[qe].indirect_dma_start(
                out=out, out_offset=None, in_=in_,
                in_offset=bass.IndirectOffsetOnAxis(ap=idx, axis=0))
        inst.then_inc(tk["sem"], 16)
        tk["cnt"] += 1
        self._record(nm, tk["cnt"], reads, writes)
        self.ninst += 1
        return inst

    def barrier(self, include_cv=False):
        for e in self.E:
            deps = {t: tk["cnt"] for t, tk in self.tok.items()
                    if tk["cnt"] > 0 and (include_cv or not t.startswith("d_cv"))}
            self._wait(e, deps)
        self.lastw = {k_: v for k_, v in self.lastw.items() if k_ in self.keep}
        self.reads = {}

    def final_wait(self):
        deps = {t: tk["cnt"] for t, tk in self.tok.items() if tk["cnt"] > 0}
        self._wait("sp", deps)

    def mm(self, out, lhsT, rhs, start, stop, reads, writes, sig=None):
        if sig is None:
            sig = stop
        return self.op("pe", lambda e: e.matmul(out, lhsT, rhs, start=start, stop=stop),
                       reads=reads, writes=writes, sig=sig)

    def tr(self, out, in_, ident, reads, writes, sig=True):
        return self.op("pe", lambda e: e.transpose(out, in_, ident), reads=reads, writes=writes, sig=sig)

    def tt(self, e, out, in0, in1, op, reads, writes):
        return self.op(e, lambda g: g.tensor_tensor(out=out, in0=in0, in1=in1, op=op), reads=reads, writes=writes)

    def ts(self, e, out, in0, s1, s2, op0, op1, reads, writes):
        if op1 is None:
            return self.op(e, lambda g: g.tensor_scalar(out=out, in0=in0, scalar1=s1, scalar2=None, op0=op0),
                           reads=reads, writes=writes)
        return self.op(e, lambda g: g.tensor_scalar(out=out, in0=in0, scalar1=s1, scalar2=s2, op0=op0, op1=op1),
                       reads=reads, writes=writes)

    def stt(self, out, in0, scalar, in1, op0, op1, reads, writes, accum_out=None):
        if accum_out is None:
            return self.op("dve", lambda g: g.scalar_tensor_tensor(out=out, in0=in0, scalar=scalar, in1=in1,
                                                                    op0=op0, op1=op1), reads=reads, writes=writes)
        return self.op("dve", lambda g: g.scalar_tensor_tensor(out=out, in0=in0, scalar=scalar, in1=in1,
                                                                op0=op0, op1=op1, accum_out=accum_out),
                       reads=reads, writes=writes)

    def cp(self, e, out, in_, reads, writes):
        return self.op(e, lambda g: g.tensor_copy(out=out, in_=in_), reads=reads, writes=writes)

    def act(self, out, in_, func, reads, writes, bias=None, scale=None, accum_out=None):
        kw = {}
        if bias is not None:
            kw["bias"] = bias
        if scale is not None:
            kw["scale"] = scale
        if accum_out is not None:
            kw["accum_out"] = accum_out
        return self.op("act", lambda g: g.activation(out=out, in_=in_, func=func, **kw), reads=reads, writes=writes)

    def memset(self, e, ap, val, writes):
        return self.op(e, lambda g: g.memset(ap, val), reads=(), writes=writes)


def build_program(stop_after="E", dbg=False):
    nc = bass.Bass("TRN2", target_bir_lowering=False)
    phases = "ABCDE"
    last = phases.index(stop_after)

    def din(name, shape, dt=F32):
        return nc.dram_tensor(name, list(shape), dt, kind="ExternalInput").ap()

    def dscr(name, shape, dt):
        return nc.dram_tensor(name, list(shape), dt, kind=("ExternalOutput" if dbg else "Internal")).ap()

    def dout(name, shape, dt=F32):
        return nc.dram_tensor(name, list(shape), dt, kind="ExternalOutput").ap()

    xs = din("xs", [TSEQ, D])
    cT = din("cT", [128, 32])
    w_mod = din("w_mod", [D, 6 * D])
    bmod_bc = din("bmod_bc", [128, 6 * D])
    n1T = din("n1T", [128, 16])
    n2_bc = din("n2_bc", [128, D])
    fw_bc = din("fw_bc", [128, D])
    w_in = din("w_in", [D, 6144])
    ident_in = din("ident", [128, 128])
    pm2_in = din("pm2", [128, 4 * 128])
    poolw_in = din("poolw", [1024, 256])
    pscT = din("pscT", [128, 8])
    e0_in = din("e0", [128, 1])
    if last >= 2:
        tidx_in = din("tidx", [128, TSEQ])
        s5_arep = din("s5_arep", [128, 128])
        s5_aimp = din("s5_aimp", [128, 128])
        s5_ldtp = din("s5_ldtp", [128, 128])
        s5_aref = din("s5_aref", [128, 16 * 128])
        s5_aimf = din("s5_aimf", [128, 16 * 128])
        s5_ldtf = din("s5_ldtf", [128, 16 * 128])
        s5_brt = din("s5_brt", [128, 16 * 128])
        s5_bst = din("s5_bst", [128, 16 * 128])
        s5_l1 = din("s5_l1", [128, 128 * 16])
        s5_l2 = din("s5_l2", [128, 128 * 16])
        sgn_in = din("sgn", [128, 4 + 128])
        gmask_in = din("gmask", [128, 8])
        dskT = din("dskT", [128, 8])
        bgluT = din("bgluT", [128, 8])
        w_glu = din("w_glu", [1024, 1024])
    if last >= 3:
        proj_a = din("proj_a", [1024, D])
        proj_b = din("proj_b", [1024, D])
        w_out = din("w_out", [D, D])
    if last >= 4:
        wq = din("wq", [D, D])
        k1T = din("k1T", [128, 128])
        k2T = din("k2T", [128, 128])
        peer_u = din("peer_u", [16384, D])
        peer_v = din("peer_v", [16384, D])
        iota16_in = din("iota16", [128, 16])
        y_out = dout("y", [NOWN, D])

    uT_d = dscr("uT_d", [8, 128, TSEQ], BF16)
    rows_d = dscr("rows_d", [4, 128, D], F32)
    ybT_d = dscr("ybT_d", [8, 128, NOWN], BF16)
    hT_d = nc.dram_tensor("hT_d", [16, 128, NOWN], BF16, kind="Internal").ap()
    if last >= 3:
        x1_d = dscr("x1_d", [NOWN, D], F32)
        h2b_d = dscr("h2b_d", [NOWN, D], BF16)
    if last >= 3:
        w16g_d = nc.dram_tensor("w16g_d", [D, 4096], BF16, kind="Internal").ap()
        pa16_d = nc.dram_tensor("pa16_d", [1024, D], BF16, kind="Internal").ap()
        pb16_d = nc.dram_tensor("pb16_d", [1024, D], BF16, kind="Internal").ap()
        wo16_d = nc.dram_tensor("wo16_d", [D, D], BF16, kind="Internal").ap()
    if last >= 4:
        uv16_d = nc.dram_tensor("uv16_d", [16384, 2 * D], BF16, kind="Internal").ap()
    if dbg:
        dbg_feat = dout("dbg_feat", [128, 64])
        dbg_rows = dout("dbg_rows", [4, D])
        if last >= 2:
            dbg_ya = dout("dbg_ya", [8, 128, NOWN], BF16)
            dbg_g = dout("dbg_g", [8, 128, NOWN], BF16)

    es = contextlib.ExitStack()
    with es:
        k = K(nc, es)
        feat = k.sb(es, "feat", [128, 64], F32)
        wm1 = k.sb(es, "wm1", [128, 32], F32)
        ident_f = k.sb(es, "ident_f", [128, 128], F32)
        ident_b = k.sb(es, "ident_b", [128, 128], BF16)
        negpi = k.sb(es, "negpi", [128, 1], F32)
        e0 = k.sb(es, "e0", [128, 1], F32)
        n1Ts = k.sb(es, "n1Ts", [128, 16], F32)

        k.dma("sp", ident_f[:], ident_in[:, :], writes=["ident_f"])
        k.dma("sp", e0[:], e0_in[:, :], writes=["e0"])
        k.dma("sp", n1Ts[:], n1T[:, :], writes=["n1Ts"])
        k.cp("dve", ident_b[:], ident_f[:], reads=["ident_f"], writes=["ident_b"])
        k.memset("dve", negpi[:], -math.pi, writes=["negpi"])
        halfpi = k.sb(es, "halfpi", [128, 1], F32)
        k.memset("dve", halfpi[:], math.pi / 2, writes=["halfpi"])
        epsc = k.sb(es, "epsc", [128, 1], F32)
        k.memset("dve", epsc[:], EPS, writes=["epsc"])
        magp = k.sb(es, "magp", [128, 1], F32)
        magn = k.sb(es, "magn", [128, 1], F32)
        k.memset("dve", magp[:], MAGIC, writes=["magp"])
        k.memset("dve", magn[:], -MAGIC, writes=["magn"])

        with contextlib.ExitStack() as pa:
            cTs = k.sb(pa, "cTs", [128, 32], F32)
            sil = k.sb(pa, "sil", [128, 32], F32)
            silbc = k.sb(pa, "silbc", [128, 32 * 128], F32)
            NWA = 4
            wst = [k.sb(pa, "wst%d" % i, [128, 4096], F32) for i in range(NWA)]
            bst = k.sb(pa, "bst", [128, 4096], F32)
            rowt = k.sb(pa, "rowt", [128, 4096], F32)
            psA = k.ps(pa, "psA", [128, 4096], F32)
            g1bc = k.sb(pa, "g1bc", [128, D], F32)
            sh2bc = k.sb(pa, "sh2bc", [128, D], F32)
            wm2bc = k.sb(pa, "wm2bc", [128, D], F32)
            g2bc = k.sb(pa, "g2bc", [128, D], F32)
            k.dma("sp", cTs[:], cT[:, :], writes=["cTs"])
            k.act(sil[:], cTs[:], AF.Silu, reads=["cTs"], writes=["sil"])
            k.cp("dve", mk_ap(silbc, 0, [[128, 32], [1, 128]]), mk_ap(sil, 0, [[1, 32], [0, 128]]),
                 reads=["sil"], writes=["silbc"])
            nld = 0
            for hh in range(2):
                c0 = hh * 2048
                k.dma("sp", bst[:, 0:2048], bmod_bc[:, c0:c0 + 2048], writes=["bst"])
                for kc in range(16):
                    wb = wst[nld % NWA]
                    wk = "wst%d" % (nld % NWA)
                    nld += 1
                    k.dma("sp", wb[:, 0:2048], w_mod[kc * 128:(kc + 1) * 128, c0:c0 + 2048], writes=[wk])
                    for v in range(2):
                        for nb in range(4):
                            bk = v * 4 + nb
                            k.mm(psA[:, bk * 512:(bk + 1) * 512],
                                 silbc[:, (v * 16 + kc) * 128:(v * 16 + kc + 1) * 128],
                                 wb[:, nb * 512:(nb + 1) * 512],
                                 start=(kc == 0), stop=(kc == 15),
                                 reads=[wk, "silbc"], writes=["psA%d" % bk], sig=(bk == 7))
                pkeys = ["psA%d" % nb for nb in range(8)]
                for v in range(2):
                    k.tt("dve", rowt[:, v * 2048:(v + 1) * 2048], psA[:, v * 2048:(v + 1) * 2048], bst[:, 0:2048],
                         ALU.add, reads=pkeys + ["bst"], writes=["rowt"])
                psF = psA
                for v in range(2):
                    for ch in range(16):
                        col = v * 32 + hh * 16 + ch
                        k.mm(psF[:, col:col + 1], rowt[:, v * 2048 + ch * 128:v * 2048 + (ch + 1) * 128], e0[:, 0:1],
                             start=True, stop=True, reads=["rowt", "e0"], writes=["psA0"],
                             sig=(v == 1 and ch == 15))
                for v in range(2):
                    k.cp("dve", feat[:, v * 32 + hh * 16:v * 32 + hh * 16 + 16],
                         psF[:, v * 32 + hh * 16:v * 32 + hh * 16 + 16], reads=["psA0"], writes=["feat"])
            for v in range(2):
                k.stt(wm1[:, v * 16:(v + 1) * 16], feat[:, v * 32 + 16:v * 32 + 32], 1.0, n1Ts[:],
                      ALU.add, ALU.mult, reads=["feat", "n1Ts"], writes=["wm1"])
            passes = [(1, 0), (2, 0)]
            for pi, (th, v) in enumerate(passes):
                c0 = th * 4096
                k.dma("sp", bst[:], bmod_bc[:, c0:c0 + 4096], writes=["bst"])
                for kc in range(16):
                    wb = wst[nld % NWA]
                    wk = "wst%d" % (nld % NWA)
                    nld += 1
                    k.dma("sp", wb[:], w_mod[kc * 128:(kc + 1) * 128, c0:c0 + 4096], writes=[wk])
                    for nb in range(8):
                        k.mm(psA[:, nb * 512:(nb + 1) * 512],
                             silbc[:, (v * 16 + kc) * 128:(v * 16 + kc + 1) * 128],
                             wb[:, nb * 512:(nb + 1) * 512],
                             start=(kc == 0), stop=(kc == 15),
                             reads=[wk, "silbc"], writes=["psA%d" % nb], sig=(nb == 7))
                pkeys = ["psA%d" % nb for nb in range(8)]
                if th == 0:
                    pass
                elif th == 1:
                    k.tt("dve", g1bc[:], psA[:, 0:2048], bst[:, 0:2048], ALU.add,
                         reads=pkeys + ["bst"], writes=["g1bc"])
                    k.tt("dve", sh2bc[:], psA[:, 2048:4096], bst[:, 2048:4096], ALU.add,
                         reads=pkeys + ["bst"], writes=["sh2bc"])
                else:
                    k.tt("dve", rowt[:, 0:2048], psA[:, 0:2048], bst[:, 0:2048], ALU.add,
                         reads=pkeys + ["bst"], writes=["rowt"])
                    k.tt("dve", g2bc[:], psA[:, 2048:4096], bst[:, 2048:4096], ALU.add,
                         reads=pkeys + ["bst"], writes=["g2bc"])
                    k.dma("sp", bst[:, 0:2048], n2_bc[:, :], reads=[], writes=["bst"])
                    k.stt(wm2bc[:], rowt[:, 0:2048], 1.0, bst[:, 0:2048], ALU.add, ALU.mult,
                          reads=["rowt", "bst"], writes=["wm2bc"])
            for ri_, (t_, tk_) in enumerate(((g1bc, "g1bc"), (sh2bc, "sh2bc"), (wm2bc, "wm2bc"), (g2bc, "g2bc"))):
                k.dma("sp", rows_d[ri_, :, :], t_[:], reads=[tk_], writes=["rows_d"])
            if dbg:
                k.dma("sp", dbg_feat[:, :], feat[:], reads=["feat"])
                k.dma("sp", dbg_rows[0:1, :], g1bc[0:1, :], reads=["g1bc"])
                k.dma("sp", dbg_rows[1:2, :], sh2bc[0:1, :], reads=["sh2bc"])
                k.dma("sp", dbg_rows[2:3, :], wm2bc[0:1, :], reads=["wm2bc"])
                k.dma("sp", dbg_rows[3:4, :], g2bc[0:1, :], reads=["g2bc"])
            k.barrier()

        def norm_tile(src_rows, xt, xtk, ss, rstd, xn, pT, hT_dst, vsel, junk):
            k.dma("sp", xt[:], src_rows, writes=[xtk])
            k.act(junk[:], xt[:], AF.Square, reads=[xtk], writes=["junk", "ss"], accum_out=ss[:, 0:1])
            k.act(rstd[:, 0:1], ss[:, 0:1], AF.Ln, reads=["ss", "epsc"], writes=["rstd"], scale=1.0 / D, bias=epsc[:, 0:1])
            k.act(rstd[:, 0:1], rstd[:, 0:1], AF.Exp, reads=["rstd"], writes=["rstd"], scale=-0.5)
            k.act(xn[:], xt[:], AF.Copy, reads=[xtk, "rstd"], writes=["xn"], scale=rstd[:, 0:1])
            for kc in range(16):
                k.tr(pT[:, kc * 128:(kc + 1) * 128], xn[:, kc * 128:(kc + 1) * 128], ident_b[:],
                     reads=["xn", "ident_b"], writes=["pT"], sig=(kc == 15))
            for kc in range(16):
                dst, dk = hT_dst(kc)
                k.act(dst, pT[:, kc * 128:(kc + 1) * 128], AF.Identity, reads=["pT", "wm1", "feat"], writes=[dk],
                      scale=wm1[:, vsel * 16 + kc:vsel * 16 + kc + 1],
                      bias=feat[:, vsel * 32 + kc:vsel * 32 + kc + 1])

        def load_cast(pool_es, dst_bf, dkey, src_ap, ncols, stage, skeys, cnt):
            sb_ = stage[cnt[0] % len(stage)]
            sk = skeys[cnt[0] % len(stage)]
            cnt[0] += 1
            k.dma("sp", sb_[:, 0:ncols], src_ap, writes=[sk])
            if cnt[0] % 2 == 0:
                k.act(dst_bf, sb_[:, 0:ncols], AF.Copy, reads=[sk], writes=[dkey])
            else:
                k.cp("dve", dst_bf, sb_[:, 0:ncols], reads=[sk], writes=[dkey])

        if last >= 1:
            with contextlib.ExitStack() as pb:
                win = k.sb(pb, "win", [128, 16 * 2048], BF16)
                stage = [k.sb(pb, "stg%d" % i, [128, 2048], F32) for i in range(4)]
                skeys = ["stg%d" % i for i in range(4)]
                cnt = [0]
                poolw = k.sb(pb, "poolw", [128, 8 * 256], BF16)
                pm2 = k.sb(pb, "pm2", [128, 4 * 128], BF16)
                psc = k.sb(pb, "psc", [128, 8], F32)
                xt = [k.sb(pb, "xt%d" % i, [128, D], F32) for i in range(2)]
                junk = k.sb(pb, "junk", [128, D], BF16)
                ss = k.sb(pb, "ss", [128, 1], F32)
                rstd = k.sb(pb, "rstd", [128, 1], F32)
                xn = k.sb(pb, "xn", [128, D], BF16)
                hTs = [k.sb(pb, "hT%d" % i, [128, 16 * 512], BF16) for i in range(2)]
                ublk = k.sb(pb, "ublk", [128, 8 * 512], BF16)
                uptm = k.sb(pb, "uptm", [128, 1024], BF16)
                pTs = k.sb(pb, "pTs", [128, 8 * 128], BF16)
                yblk = k.sb(pb, "yblk", [128, 8 * 512], BF16)
                pT = k.ps(pb, "pT", [128, D], BF16)
                pU = [k.ps(pb, "pU%d" % i, [128, 512], F32) for i in range(2)]
                pP = k.ps(pb, "pP", [128, 1024], F32)
                pQ = k.ps(pb, "pQ", [128, 1024], F32)

                for kc in range(16):
                    load_cast(pb, win[:, kc * 2048:(kc + 1) * 2048], "win", w_in[kc * 128:(kc + 1) * 128, 0:2048],
                              2048, stage, skeys, cnt)
                for r in range(8):
                    load_cast(pb, poolw[:, r * 256:(r + 1) * 256], "poolw", poolw_in[r * 128:(r + 1) * 128, :],
                              256, stage, skeys, cnt)
                load_cast(pb, pm2[:], "pm2", pm2_in[:, :], 512, stage, skeys, cnt)
                k.dma("sp", psc[:], pscT[:, :], writes=["psc"])
                k.barrier()

                blocks = [(SEQ, 256, 1, False)] + [(i * 512, 512, 0, i < 4) for i in range(8)]
                nx = 0
                nu = 0
                for bi_, (row0, ntok, vsel, own) in enumerate(blocks):
                    ntile = ntok // 128
                    hT = hTs[bi_ % 2]
                    hk = "hT%d" % (bi_ % 2)
                    for j in range(ntile):
                        xb = xt[nx % 2]
                        xk = "xt%d" % (nx % 2)
                        nx += 1
                        norm_tile(xs[row0 + j * 128:row0 + (j + 1) * 128, :], xb, xk, ss, rstd, xn, pT,
                                  lambda kc, j=j: (hT[:, kc * 512 + j * 128:kc * 512 + (j + 1) * 128], hk),
                                  vsel, junk)
                    for ct in range(8):
                        pu = pU[nu % 2]
                        puk = "pU%d" % (nu % 2)
                        nu += 1
                        for kc in range(16):
                            k.mm(pu[:, 0:ntok], win[:, kc * 2048 + ct * 128:kc * 2048 + (ct + 1) * 128],
                                 hT[:, kc * 512:kc * 512 + ntok], start=(kc == 0), stop=(kc == 15),
                                 reads=["win", hk], writes=[puk])
                        k.cp("dve", ublk[:, ct * 512:ct * 512 + ntok], pu[:, 0:ntok], reads=[puk], writes=["ublk"])
                    k.dma("sp", uT_d[:, :, row0:row0 + ntok].rearrange("c p t -> p c t"),
                          mk_ap(ublk, 0, [[512, 8], [1, ntok]]), reads=["ublk"], writes=["uT_d"])
                    if not own:
                        continue
                    k.dma("sp", hT_d[:, :, row0:row0 + 512].rearrange("c p t -> p c t"),
                          mk_ap(hT, 0, [[512, 16], [1, 512]]), reads=[hk], writes=["hT_d"])
                    for j in range(4):
                        for hf in range(2):
                            for kc in range(16):
                                k.mm(pP[:, hf * 512:(hf + 1) * 512], hT[:, kc * 512 + j * 128:kc * 512 + (j + 1) * 128],
                                     win[:, kc * 2048 + 1024 + hf * 512:kc * 2048 + 1024 + (hf + 1) * 512],
                                     start=(kc == 0), stop=(kc == 15), reads=["win", hk], writes=["pP%d" % hf])
                        k.act(uptm[:], pP[:], AF.Copy, reads=["pP0", "pP1"], writes=["uptm"])
                        for ct in range(8):
                            g = ct // 2
                            k.mm(pQ[:, ct * 128:(ct + 1) * 128], uptm[:, ct * 128:(ct + 1) * 128],
                                 pm2[:, g * 128:(g + 1) * 128], start=True, stop=True,
                                 reads=["uptm", "pm2"], writes=["pQ"], sig=(ct == 7))
                        k.cp("dve", pTs[:], pQ[:], reads=["pQ"], writes=["pTs"])
                        for cot in range(8):
                            g = cot // 2
                            hf = cot % 2
                            for cit in range(2):
                                k.mm(pQ[:, cot * 128:(cot + 1) * 128],
                                     poolw[:, (g * 2 + cit) * 256 + hf * 128:(g * 2 + cit) * 256 + (hf + 1) * 128],
                                     pTs[:, (g * 2 + cit) * 128:(g * 2 + cit + 1) * 128],
                                     start=(cit == 0), stop=(cit == 1), reads=["pTs", "poolw"], writes=["pQ"],
                                     sig=(cot == 7 and cit == 1))
                        k.tt("dve", mk_ap(yblk, j * 128, [[512, 8], [1, 128]]), mk_ap(pQ, 0, [[128, 8], [1, 128]]),
                             mk_ap(psc, 0, [[1, 8], [0, 128]]), ALU.mult, reads=["pQ", "psc"], writes=["yblk"])
                    k.dma("sp", ybT_d[:, :, row0:row0 + 512].rearrange("c p t -> p c t"),
                          mk_ap(yblk, 0, [[512, 8], [1, 512]]), reads=["yblk"], writes=["ybT_d"])
                k.barrier()

        if last >= 2:
            pcd = contextlib.ExitStack()
            es.callback(pcd.close)
            yaT = k.sb(pcd, "yaT", [128, 8 * NOWN], BF16)
            cv_jobs = []
            if last >= 3:
                k.keep.update(["w16"])
                for r_ in (0, 1024):
                    for ch_ in (0, 2048):
                        cv_jobs.append((w16g_d[r_:r_ + 1024, ch_:ch_ + 2048],
                                        w_in[r_:r_ + 1024, 2048 + ch_:2048 + ch_ + 2048], "w16"))
                cv_jobs.append((pa16_d[:, :], proj_a[:, :], "w16"))
                cv_jobs.append((pb16_d[:, :], proj_b[:, :], "w16"))
                for r_ in (0, 1024):
                    cv_jobs.append((wo16_d[r_:r_ + 1024, :], w_out[r_:r_ + 1024, :], "w16"))
            if last >= 4:
                CH = 1024
                k.keep.update(["u16_d", "v16_d"])
                for r_ in range(0, 16384, CH):
                    cv_jobs.append((uv16_d[r_:r_ + CH, 0:D], peer_u[r_:r_ + CH, :], "u16_d"))
                    cv_jobs.append((uv16_d[r_:r_ + CH, D:2 * D], peer_v[r_:r_ + CH, :], "v16_d"))
            with contextlib.ExitStack() as pc:
                B1b = k.sb(pc, "B1b", [128, 2048], BF16)
                B2b = k.sb(pc, "B2b", [128, 2048], BF16)
                rcb = k.sb(pc, "rcb", [128, 2048], BF16)
                rc2b = k.sb(pc, "rc2b", [128, 2048], BF16)
                Rp = k.sb(pc, "Rp", [128, 128], F32)
                w2p = k.sb(pc, "w2p", [128, 128], F32)
                gmask = k.sb(pc, "gmask", [128, 8], F32)
                gmaskn = k.sb(pc, "gmaskn", [128, 8], F32)
                dsk = k.sb(pc, "dsk", [128, 8], F32)
                bglu = k.sb(pc, "bglu", [128, 8], F32)
                k.dma("sp", gmask[:], gmask_in[:, :], writes=["gmask"])
                k.dma("sp", dsk[:], dskT[:, :], writes=["dsk"])
                k.dma("sp", bglu[:], bgluT[:, :], writes=["bglu"])
                k.ts("dve", gmaskn[:], gmask[:], -1.0, None, ALU.mult, None, reads=["gmask"], writes=["gmaskn"])
                with contextlib.ExitStack() as pp:
                    nm5 = ["aref", "aimf", "ldtf", "brt", "bst", "l1", "l2"]
                    T = {n: k.sb(pp, "c_" + n, [128, 2048], F32) for n in nm5}
                    for n, src in zip(nm5, [s5_aref, s5_aimf, s5_ldtf, s5_brt, s5_bst, s5_l1, s5_l2]):
                        k.dma("sp", T[n][:], src[:, :], writes=["c_" + n])
                    sgnf = k.sb(pp, "sgnf", [128, 128], F32)
                    sgn = k.sb(pp, "sgn", [128, 4], F32)
                    k.dma("sp", sgnf[:], sgn_in[:, 4:132], writes=["sgnf"])
                    k.dma("sp", sgn[:], sgn_in[:, 0:4], writes=["sgn"])
                    q = [k.sb(pp, "q%d" % i, [128, 128], F32) for i in range(4)]
                    for i_, src in enumerate([s5_arep, s5_aimp, s5_ldtp]):
                        k.dma("sp", q[i_][:], src[:, :], writes=["q%d" % i_])
                    k.act(q[3][:], q[2][:], AF.Exp, reads=["q2"], writes=["q3"])
                    k.tt("dve", q[0][:], q[3][:], q[0][:], ALU.mult, reads=["q3", "q0"], writes=["q0"])
                    k.act(Rp[:], q[0][:], AF.Exp, reads=["q0"], writes=["Rp"])
                    k.tt("dve", q[1][:], q[3][:], q[1][:], ALU.mult, reads=["q3", "q1"], writes=["q1"])
                    k.ts("dve", q[1][:], q[1][:], INV_2PI, None, ALU.mult, None, reads=["q1"], writes=["q1"])
                    k.ts("dve", q[2][:], q[1][:], MAGIC, MAGIC, ALU.add, ALU.subtract, reads=["q1"], writes=["q2"])
                    k.tt("dve", w2p[:], q[1][:], q[2][:], ALU.subtract, reads=["q1", "q2"], writes=["w2p"])
                    t = [k.sb(pp, "t%d" % i, [128, 2048], F32) for i in range(6)]
                    tk = ["t%d" % i for i in range(6)]
                    aref, aimf, ldtf, brt, bst_ = T["aref"], T["aimf"], T["ldtf"], T["brt"], T["bst"]
                    k.act(t[0][:], ldtf[:], AF.Exp, reads=["c_ldtf"], writes=[tk[0]])
                    k.tt("dve", t[1][:], t[0][:], aref[:], ALU.mult, reads=[tk[0], "c_aref"], writes=[tk[1]])
                    k.act(t[1][:], t[1][:], AF.Exp, reads=[tk[1]], writes=[tk[1]])
                    k.tt("dve", t[2][:], t[0][:], aimf[:], ALU.mult, reads=[tk[0], "c_aimf"], writes=[tk[2]])
                    k.ts("dve", t[2][:], t[2][:], INV_2PI, None, ALU.mult, None, reads=[tk[2]], writes=[tk[2]])
                    k.ts("dve", t[3][:], t[2][:], MAGIC, MAGIC, ALU.add, ALU.subtract, reads=[tk[2]], writes=[tk[3]])
                    k.tt("dve", t[2][:], t[2][:], t[3][:], ALU.subtract, reads=[tk[2], tk[3]], writes=[tk[2]])
                    k.act(t[3][:], t[2][:], AF.Sin, reads=[tk[2]], writes=[tk[3]], scale=TWO_PI_S)
                    k.act(t[2][:], t[2][:], AF.Abs, reads=[tk[2]], writes=[tk[2]])
                    k.act(t[2][:], t[2][:], AF.Sin, reads=[tk[2], "halfpi"], writes=[tk[2]], scale=-TWO_PI,
                          bias=halfpi[:, 0:1])
                    k.tt("dve", t[2][:], t[1][:], t[2][:], ALU.mult, reads=[tk[1], tk[2]], writes=[tk[2]])
                    k.tt("dve", t[3][:], t[1][:], t[3][:], ALU.mult, reads=[tk[1], tk[3]], writes=[tk[3]])
                    k.ts("dve", t[2][:], t[2][:], -1.0, None, ALU.add, None, reads=[tk[2]], writes=[tk[2]])
                    k.tt("dve", t[0][:], aref[:], aref[:], ALU.mult, reads=["c_aref"], writes=[tk[0]])
                    k.tt("dve", t[1][:], aimf[:], aimf[:], ALU.mult, reads=["c_aimf"], writes=[tk[1]])
                    k.tt("dve", t[0][:], t[0][:], t[1][:], ALU.add, reads=[tk[0], tk[1]], writes=[tk[0]])
                    k.op("dve", lambda g_: g_.reciprocal(out=t[0][:], in_=t[0][:]), reads=[tk[0]], writes=[tk[0]])
                    k.tt("dve", t[1][:], t[2][:], aref[:], ALU.mult, reads=[tk[2], "c_aref"], writes=[tk[1]])
                    k.tt("dve", t[4][:], t[3][:], aimf[:], ALU.mult, reads=[tk[3], "c_aimf"], writes=[tk[4]])
                    k.tt("dve", t[1][:], t[1][:], t[4][:], ALU.add, reads=[tk[1], tk[4]], writes=[tk[1]])
                    k.tt("dve", t[1][:], t[1][:], t[0][:], ALU.mult, reads=[tk[1], tk[0]], writes=[tk[1]])
                    k.tt("dve", t[4][:], t[3][:], aref[:], ALU.mult, reads=[tk[3], "c_aref"], writes=[tk[4]])
                    k.tt("dve", t[5][:], t[2][:], aimf[:], ALU.mult, reads=[tk[2], "c_aimf"], writes=[tk[5]])
                    k.tt("dve", t[4][:], t[4][:], t[5][:], ALU.subtract, reads=[tk[4], tk[5]], writes=[tk[4]])
                    k.tt("dve", t[4][:], t[4][:], t[0][:], ALU.mult, reads=[tk[4], tk[0]], writes=[tk[4]])
                    sg_bc = mk_ap(sgnf, 0, [[0, 16], [1, 128]])

                    def v3(tt_):
                        return mk_ap(tt_, 0, [[128, 16], [1, 128]])
                    k.tt("dve", v3(t[5]), v3(t[4]), sg_bc, ALU.mult, reads=[tk[4], "sgnf"], writes=[tk[5]])
                    k.tt("dve", t[5][:], t[5][:], bst_[:], ALU.mult, reads=[tk[5], "c_bst"], writes=[tk[5]])
                    k.tt("dve", t[2][:], t[1][:], brt[:], ALU.mult, reads=[tk[1], "c_brt"], writes=[tk[2]])
                    k.tt("dve", B1b[:], t[2][:], t[5][:], ALU.add, reads=[tk[2], tk[5]], writes=["B1b"])
                    k.tt("dve", v3(t[2]), v3(t[1]), sg_bc, ALU.mult, reads=[tk[1], "sgnf"], writes=[tk[2]])
                    k.tt("dve", t[2][:], t[2][:], bst_[:], ALU.mult, reads=[tk[2], "c_bst"], writes=[tk[2]])
                    k.tt("dve", t[5][:], t[4][:], brt[:], ALU.mult, reads=[tk[4], "c_brt"], writes=[tk[5]])
                    k.tt("dve", B2b[:], t[2][:], t[5][:], ALU.subtract, reads=[tk[2], tk[5]], writes=["B2b"])
                    k.ts("dve", rcb[:], T["l1"][:], sgn[:, 0:1], None, ALU.mult, None, reads=["c_l1", "sgn"], writes=["rcb"])
                    k.ts("dve", rc2b[:, 0:1024], T["l2"][:, 0:1024], -1.0, None, ALU.mult, None, reads=["c_l2"], writes=["rc2b"])
                    k.cp("dve", rc2b[:, 1024:2048], T["l2"][:, 1024:2048], reads=["c_l2"], writes=["rc2b"])
                    k.barrier()

                gT = k.sb(pc, "gT", [128, 8 * NOWN], BF16)
                tidx = k.sb(pc, "tidx", [128, TSEQ], F32)
                k.dma("sp", tidx[:], tidx_in[:, :], writes=["tidx"])
                pcm_cm = contextlib.ExitStack()
                with pcm_cm as pcm:
                    uct = [k.sb(pcm, "uct%d" % i, [128, TSEQ], BF16) for i in range(2)]
                    lb1s = [k.sb(pcm, "lb1_%d" % i, [128, 2048], BF16) for i in range(2)]
                    lb2s = [k.sb(pcm, "lb2_%d" % i, [128, 2048], BF16) for i in range(2)]
                    lrcs = [k.sb(pcm, "lrc_%d" % i, [128, 2048], BF16) for i in range(2)]
                    lrc2s = [k.sb(pcm, "lrc2_%d" % i, [128, 2048], BF16) for i in range(2)]
                    NR = 4

                    def mkring(name, dt_, depth):
                        return [k.sb(pcm, "%s%d" % (name, i), [128, 512], dt_) for i in range(depth)]
                    r_a1, r_a2 = mkring("a1_", F32, NR), mkring("a2_", F32, NR)
                    r_ns, r_nc = mkring("ns_", F32, NR), mkring("nc_", F32, NR)
                    r_tp, r_tm, r_W = mkring("tp_", F32, 2), mkring("tm_", F32, 2), mkring("W_", F32, 2)
                    r_Ao, r_Bo = mkring("Ao_", BF16, 2), mkring("Bo_", BF16, 2)
                    ytmp = k.sb(pcm, "ytmp", [128, NOWN], F32)
                    py = k.ps(pcm, "py", [128, NOWN], F32)
                    pb = [k.ps(pcm, "pb%d" % i, [128, 512], F32) for i in range(2)]
                    pb2 = [k.ps(pcm, "pb2%d" % i, [128, 512], F32) for i in range(2)]
                    pykeys = ["py%d" % j for j in range(4)]
                    segsA = [(4096, 256, 0, False)] + [(j * 512, 512, 256 + j * 512, True) for j in range(4)]
                    segsB = [(4096, 256, 4096, False)] + [(j * 512, 512, j * 512, j < 4) for j in range(7, -1, -1)]

                    def prep_ct(ct):
                        par = ct % 2
                        k.dma("sp", uct[par][:], uT_d[ct, :, :], reads=["uT_d"], writes=["uct%d" % par])
                        lb1, lb2, lrc, lrc2 = lb1s[par], lb2s[par], lrcs[par], lrc2s[par]
                        for slot in range(2):
                            blk = (ct * 2 + slot) * 128
                            k.tt("dve", mk_ap(lb1, slot * 1024, [[128, 8], [1, 128]]), mk_ap(B1b, blk, [[0, 8], [1, 128]]),
                                 mk_ap(gmask, 0, [[1, 8], [0, 128]]), ALU.mult, reads=["B1b", "gmask"],
                                 writes=["lb1_%d" % par])
                            gm = gmask if slot == 0 else gmaskn
                            k.tt("dve", mk_ap(lb2, slot * 1024, [[128, 8], [1, 128]]), mk_ap(B2b, blk, [[0, 8], [1, 128]]),
                                 mk_ap(gm, 0, [[1, 8], [0, 128]]), ALU.mult, reads=["B2b", "gmask", "gmaskn"],
                                 writes=["lb2_%d" % par])
                            k.memset("dve", lrc[:, slot * 1024:(slot + 1) * 1024], 0.0, writes=["lrc_%d" % par])
                            k.cp("dve", mk_ap(lrc, slot * 1024, [[144, 8], [1, 16]]),
                                 mk_ap(rcb, (slot * 64 + ct * 8) * 16, [[16, 8], [1, 16]]), reads=["rcb"],
                                 writes=["lrc_%d" % par])
                            k.memset("dve", lrc2[:, slot * 1024:(slot + 1) * 1024], 0.0, writes=["lrc2_%d" % par])
                            k.cp("dve", mk_ap(lrc2, slot * 1024, [[144, 8], [1, 16]]),
                                 mk_ap(rc2b, (slot * 64 + ct * 8) * 16, [[16, 8], [1, 16]]), reads=["rc2b"],
                                 writes=["lrc2_%d" % par])

                    segl = []
                    for ct in range(8):
                        for slot, segs in ((0, segsA), (1, segsB)):
                            for gi in range(8):
                                for si, (c0, n, tabs0, own) in enumerate(segs):
                                    segl.append(dict(ct=ct, slot=slot, gi=gi, c0=c0, n=n, tabs0=tabs0, own=own,
                                                     first=(si == 0), col=slot * 64 + ct * 8 + gi,
                                                     wo=slot * 1024 + gi * 128, q=len(segl), last_of_ct=False))
                        segl[-1]["last_of_ct"] = True
                    chain = {}

                    def kk_(nm, sg):
                        return "%s%d" % (nm, sg["q"] % NR)

                    def k2_(nm, sg):
                        return "%s%d" % (nm, sg["q"] % 2)

                    def stA(sg):
                        i, n = sg["q"] % NR, sg["n"]
                        a1, a2 = r_a1[i], r_a2[i]
                        k.act(a1[:, 0:n], tidx[:, sg["tabs0"]:sg["tabs0"] + n], AF.Copy, reads=["tidx", "w2p"],
                              writes=[kk_("a1_", sg)], scale=w2p[:, sg["col"]:sg["col"] + 1])
                        k.act(a2[:, 0:n], a1[:, 0:n], AF.Identity, reads=[kk_("a1_", sg), "magp"],
                              writes=[kk_("a2_", sg)], bias=magp[:, 0:1])
                        k.act(a2[:, 0:n], a2[:, 0:n], AF.Identity, reads=[kk_("a2_", sg), "magn"],
                              writes=[kk_("a2_", sg)], bias=magn[:, 0:1])
                        k.tt("pool", a1[:, 0:n], a1[:, 0:n], a2[:, 0:n], ALU.subtract,
                             reads=[kk_("a1_", sg), kk_("a2_", sg)], writes=[kk_("a1_", sg)])

                    def stB(sg):
                        i, n = sg["q"] % NR, sg["n"]
                        a1, a2, ns, ncs = r_a1[i], r_a2[i], r_ns[i], r_nc[i]
                        k.act(ns[:, 0:n], a1[:, 0:n], AF.Sin, reads=[kk_("a1_", sg)], writes=[kk_("ns_", sg)],
                              scale=TWO_PI_S)
                        k.act(a2[:, 0:n], a1[:, 0:n], AF.Abs, reads=[kk_("a1_", sg)], writes=[kk_("a2_", sg)])
                        k.act(ncs[:, 0:n], a2[:, 0:n], AF.Sin, reads=[kk_("a2_", sg), "halfpi"],
                              writes=[kk_("nc_", sg)], scale=-TWO_PI, bias=halfpi[:, 0:1])

                    def stP(sg):
                        ip, n, c0, wo, par = sg["q"] % 2, sg["n"], sg["c0"], sg["wo"], sg["ct"] % 2
                        ub, ubk = uct[par], "uct%d" % par
                        k.mm(pb[ip][:, 0:n], lb1s[par][:, wo:wo + 128], ub[:, c0:c0 + n], True, True,
                             reads=["lb1_%d" % par, ubk], writes=["pb%d" % ip], sig=False)
                        k.mm(pb2[ip][:, 0:n], lb2s[par][:, wo:wo + 128], ub[:, c0:c0 + n], True, True,
                             reads=["lb2_%d" % par, ubk], writes=["pb2%d" % ip], sig=True)

                    def stC(sg):
                        i, ip, n, c0, wo = sg["q"] % NR, sg["q"] % 2, sg["n"], sg["c0"], sg["wo"]
                        slot, gi, col, ct, par = sg["slot"], sg["gi"], sg["col"], sg["ct"], sg["ct"] % 2
                        desc = slot == 1
                        ns, ncs = r_ns[i], r_nc[i]
                        tp, tm, W = r_tp[ip], r_tm[ip], r_W[ip]
                        k.tt("dve", tp[:, 0:n], ncs[:, 0:n], pb[ip][:, 0:n], ALU.mult,
                             reads=[kk_("nc_", sg), "pb%d" % ip], writes=[k2_("tp_", sg)])
                        yield
                        k.tt("dve", tm[:, 0:n], ns[:, 0:n], pb2[ip][:, 0:n], ALU.mult,
                             reads=[kk_("ns_", sg), "pb2%d" % ip], writes=[k2_("tm_", sg)])

                    def stC2(sg):
                        i, ip, n, c0, wo = sg["q"] % NR, sg["q"] % 2, sg["n"], sg["c0"], sg["wo"]
                        slot, gi, col, ct, par = sg["slot"], sg["gi"], sg["col"], sg["ct"], sg["ct"] % 2
                        desc = slot == 1
                        ns, ncs = r_ns[i], r_nc[i]
                        tp, tm, W = r_tp[ip], r_tm[ip], r_W[ip]
                        k.tt("dve", tp[:, 0:n], tp[:, 0:n], tm[:, 0:n], ALU.subtract,
                             reads=[k2_("tp_", sg), k2_("tm_", sg)], writes=[k2_("tp_", sg)])
                        yield
                        if not desc:
                            d1 = tp[:, 0:n]
                            o_ = W[:, 0:n]
                        else:
                            d1 = mk_ap(tp, n - 1, [[-1, n]])
                            o_ = mk_ap(W, n - 1, [[-1, n]])
                        d0 = mk_ap(Rp, col, [[0, n]])
                        if sg["first"]:
                            init, rds = 0.0, [k2_("tp_", sg), "Rp"]
                        else:
                            carry, prevWk = chain[(ct, slot, gi)]
                            init, rds = carry, [k2_("tp_", sg), "Rp", prevWk]
                        k.op("dve", lambda g_: g_.tensor_tensor_scan(out=o_, data0=d0, data1=d1, initial=init,
                                                                      op0=ALU.mult, op1=ALU.add),
                             reads=rds, writes=[k2_("W_", sg)])
                        chain[(ct, slot, gi)] = ((W[:, n - 1:n] if not desc else W[:, 0:1]), k2_("W_", sg))
                        if sg["own"]:
                            Ao, Bo = r_Ao[ip], r_Bo[ip]
                            k.tt("pool", Ao[:, 0:n], ncs[:, 0:n], W[:, 0:n], ALU.mult,
                                 reads=[kk_("nc_", sg), k2_("W_", sg)], writes=[k2_("Ao_", sg)])
                            k.tt("pool", Bo[:, 0:n], ns[:, 0:n], W[:, 0:n], ALU.mult,
                                 reads=[kk_("ns_", sg), k2_("W_", sg)], writes=[k2_("Bo_", sg)])
                            first = (slot == 0 and gi == 0)
                            lastmm = (slot == 1 and gi == 7)
                            bank = c0 // 512
                            k.mm(py[:, c0:c0 + n], lrcs[par][:, wo:wo + 128], Ao[:, 0:n], first, False,
                                 reads=["lrc_%d" % par, k2_("Ao_", sg)], writes=[pykeys[bank]], sig=False)
                            k.mm(py[:, c0:c0 + n], lrc2s[par][:, wo:wo + 128], Bo[:, 0:n], False, lastmm,
                                 reads=["lrc2_%d" % par, k2_("Bo_", sg)], writes=[pykeys[bank]], sig=True)
                        if sg["last_of_ct"]:
                            k.stt(ytmp[:], uct[par][:, 0:NOWN], dsk[:, ct:ct + 1], py[:, :], ALU.mult, ALU.add,
                                  reads=["uct%d" % par, "dsk"] + pykeys, writes=["ytmp"])
                            k.act(gT[:, ct * NOWN:(ct + 1) * NOWN], ytmp[:], AF.Gelu_apprx_tanh, reads=["ytmp"],
                                  writes=["gT"])
                            if ct + 2 < 8:
                                prep_ct(ct + 2)

                    prep_ct(0)
                    prep_ct(1)
                    L_ = len(segl)
                    def _run(gens):
                        gens = [g for g in gens if g is not None]
                        while gens:
                            for g in list(gens):
                                try:
                                    next(g)
                                except StopIteration:
                                    gens.remove(g)

                    for it in range(L_ + 4):
                        if it % 14 == 0 and cv_jobs:
                            cj = cv_jobs.pop(0)
                            k.dma("cv", cj[0], cj[1], writes=[cj[2]])
                        if 0 <= it - 2 < L_:
                            stP(segl[it - 2])
                        _run([stC(segl[it - 3]) if 0 <= it - 3 < L_ else None,
                              stC2(segl[it - 4]) if 0 <= it - 4 < L_ else None])
                        if 0 <= it - 2 < L_:
                            stB(segl[it - 2])
                        if it < L_:
                            stA(segl[it])
                    k.barrier()
                while cv_jobs:
                    cj = cv_jobs.pop(0)
                    k.dma("cv", cj[0], cj[1], writes=[cj[2]])
                wglu = k.sb(pc, "wglu", [128, 8 * 1024], BF16)
                gst = [k.sb(pc, "gst%d" % i, [128, 1024], F32) for i in range(2)]
                cntg = [0]
                for ci in range(8):
                    load_cast(pc, wglu[:, ci * 1024:(ci + 1) * 1024], "wglu", w_glu[ci * 128:(ci + 1) * 128, :], 1024,
                              gst, ["gst0", "gst1"], cntg)
                k.barrier()
                sgr = [k.sb(pc, "sg_%d" % i, [128, 512], F32) for i in range(2)]
                pb = [k.ps(pc, "pbg%d" % i, [128, 512], F32) for i in range(2)]
                ng = 0
                for cot in range(8):
                    for tb in range(4):
                        i = ng % 2
                        ng += 1
                        pg, pgk = pb[i], "pbg%d" % i
                        for ci in range(8):
                            k.mm(pg[:, :], wglu[:, ci * 1024 + cot * 128:ci * 1024 + (cot + 1) * 128],
                                 gT[:, ci * NOWN + tb * 512:ci * NOWN + (tb + 1) * 512], ci == 0, ci == 7,
                                 reads=["wglu", "gT"], writes=[pgk])
                        k.act(sgr[i][:], pg[:], AF.Sigmoid, reads=[pgk, "bglu"], writes=["sg_%d" % i],
                              bias=bglu[:, cot:cot + 1])
                        k.tt("dve", yaT[:, cot * NOWN + tb * 512:cot * NOWN + (tb + 1) * 512],
                             gT[:, cot * NOWN + tb * 512:cot * NOWN + (tb + 1) * 512], sgr[i][:], ALU.mult,
                             reads=["gT", "sg_%d" % i], writes=["yaT"])
                if dbg:
                    k.dma("sp", dbg_g[:, :, :].rearrange("c p t -> p c t"), mk_ap(gT, 0, [[NOWN, 8], [1, NOWN]]),
                          reads=["gT"])
                    k.dma("sp", dbg_ya[:, :, :].rearrange("c p t -> p c t"), mk_ap(yaT, 0, [[NOWN, 8], [1, NOWN]]),
                          reads=["yaT"])
                k.barrier(include_cv=True)

        if last >= 3:
            with contextlib.ExitStack() as pd:
                g1bc = k.sb(pd, "g1bc", [128, D], F32)
                k.dma("sp", g1bc[:], rows_d[0, :, :], reads=["rows_d"], writes=["g1bc"])
                hTd = [k.sb(pd, "hTd%d" % i, [128, 16 * 512], BF16) for i in range(2)]
                gateT = k.sb(pd, "gateT", [128, 32 * 512], BF16)
                ybb = k.sb(pd, "ybb", [128, 8 * 512], BF16)
                mT = k.sb(pd, "mT", [128, 16 * 512], BF16)
                xs4 = [k.sb(pd, "xs4_%d" % i, [128, D], F32) for i in range(4)]
                junk = k.sb(pd, "junk", [128, D], BF16)
                ss = k.sb(pd, "ss", [128, 1], F32)
                rstd = k.sb(pd, "rstd", [128, 1], F32)
                xn = k.sb(pd, "xn", [128, D], BF16)
                NW = 4
                wbf = [k.sb(pd, "wbf%d" % i, [128, 16 * 256], BF16) for i in range(NW)]
                t1 = k.sb(pd, "t1", [128, 512], F32)
                t2 = k.sb(pd, "t2", [128, 512], F32)
                pT = k.ps(pd, "pT", [128, D], BF16)
                pg = [k.ps(pd, "pg%d" % i, [128, 512], F32) for i in range(2)]
                pA = k.ps(pd, "pA", [128, 512], F32)
                pB = k.ps(pd, "pB", [128, 512], F32)
                pO = [k.ps(pd, "pO%d" % i, [128, 128], F32) for i in range(2)]
                nw = [0]
                wcache = {}

                def wunit(src16, col0, nk):
                    u_ = col0 // 256
                    ck = id(src16.tensor)
                    if wcache.get(ck, (None,))[0] != u_:
                        i = nw[0] % NW
                        nw[0] += 1
                        k.dma("sp", mk_ap(wbf[i], 0, [[256, nk], [1, 256]]),
                              src16[0:nk * 128, u_ * 256:(u_ + 1) * 256].rearrange("(kc p) c -> p kc c", p=128),
                              reads=["w16"], writes=["wbf%d" % i])
                        wcache[ck] = (u_, i)
                    i = wcache[ck][1]
                    off = col0 % 256
                    return (lambda kc: wbf[i][:, kc * 256 + off:kc * 256 + off + 128]), "wbf%d" % i

                npg = 0
                npo = 0
                for blk in range(4):
                    row0 = blk * 512
                    hT = hTd[blk % 2]
                    hkd = "hTd%d" % (blk % 2)
                    k.dma("sp", mk_ap(hT, 0, [[512, 16], [1, 512]]),
                          hT_d[:, :, row0:row0 + 512].rearrange("c p t -> p c t"), reads=["hT_d"], writes=[hkd])
                    for j in range(4):
                        k.dma("sp", xs4[j][:], xs[row0 + j * 128:row0 + (j + 1) * 128, :], writes=["xs4_%d" % j])
                    k.dma("sp", mk_ap(ybb, 0, [[512, 8], [1, 512]]),
                          ybT_d[:, :, row0:row0 + 512].rearrange("c p t -> p c t"), reads=["ybT_d"], writes=["ybb"])
                    for f in range(32):
                        wb, wk = wunit(w16g_d, f * 128, 16)
                        p_ = pg[npg % 2]
                        pk = "pg%d" % (npg % 2)
                        npg += 1
                        for kc in range(16):
                            k.mm(p_[:, :], wb(kc), hT[:, kc * 512:(kc + 1) * 512],
                                 kc == 0, kc == 15, reads=[wk, hkd], writes=[pk])
                        k.act(gateT[:, f * 512:(f + 1) * 512], p_[:, :], AF.Sigmoid, reads=[pk], writes=["gateT"])
                    for dt_ in range(16):
                        wb, wk = wunit(pa16_d, dt_ * 128, 8)
                        for ci in range(8):
                            k.mm(pA[:, :], wb(ci),
                                 yaT[:, ci * NOWN + row0:ci * NOWN + row0 + 512], ci == 0, ci == 7,
                                 reads=[wk, "yaT"], writes=["pA"])
                        wb, wk = wunit(pb16_d, dt_ * 128, 8)
                        for ci in range(8):
                            k.mm(pB[:, :], wb(ci), ybb[:, ci * 512:(ci + 1) * 512],
                                 ci == 0, ci == 7, reads=[wk, "ybb"], writes=["pB"])
                        k.tt("dve", t1[:], pA[:, :], gateT[:, dt_ * 512:(dt_ + 1) * 512], ALU.mult,
                             reads=["pA", "gateT"], writes=["t1"])
                        k.tt("dve", t2[:], pB[:, :], gateT[:, (16 + dt_) * 512:(17 + dt_) * 512], ALU.mult,
                             reads=["pB", "gateT"], writes=["t2"])
                        k.tt("dve", mT[:, dt_ * 512:(dt_ + 1) * 512], t1[:], t2[:], ALU.add,
                             reads=["t1", "t2"], writes=["mT"])
                    for dt_ in range(16):
                        wb, wk = wunit(wo16_d, dt_ * 128, 16)
                        for j in range(4):
                            po = pO[npo % 2]
                            pok = "pO%d" % (npo % 2)
                            npo += 1
                            for kc in range(16):
                                k.mm(po[:, :], mT[:, kc * 512 + j * 128:kc * 512 + (j + 1) * 128],
                                     wb(kc), kc == 0, kc == 15,
                                     reads=[wk, "mT"], writes=[pok])
                            k.tt("dve", t1[:, 0:128], po[:, :], g1bc[:, dt_ * 128:(dt_ + 1) * 128], ALU.mult,
                                 reads=[pok, "g1bc"], writes=["t1"])
                            k.tt("dve", xs4[j][:, dt_ * 128:(dt_ + 1) * 128], xs4[j][:, dt_ * 128:(dt_ + 1) * 128],
                                 t1[:, 0:128], ALU.add, reads=["t1", "xs4_%d" % j], writes=["xs4_%d" % j])
                    for j in range(4):
                        k.dma("sp", x1_d[row0 + j * 128:row0 + (j + 1) * 128, :], xs4[j][:],
                              reads=["xs4_%d" % j], writes=["x1_d"])
                k.barrier()
            pcd.close()

        if last >= 4:
            with contextlib.ExitStack() as pe_:
                NT = NOWN // 128
                eid_all = k.sb(pe_, "eid_all", [128, NT * 128], U32)
                gw_all = k.sb(pe_, "gw_all", [128, NT * 128], F32)
                ss = k.sb(pe_, "ss", [128, 1], F32)
                rstd = k.sb(pe_, "rstd", [128, 1], F32)

                def rms_rstd(src, srck, junk_t, junkk):
                    k.act(junk_t[:], src[:], AF.Square, reads=[srck], writes=[junkk, "ss"], accum_out=ss[:, 0:1])
                    k.act(rstd[:, 0:1], ss[:, 0:1], AF.Ln, reads=["ss", "epsc"], writes=["rstd"], scale=1.0 / D,
                          bias=epsc[:, 0:1])
                    k.act(rstd[:, 0:1], rstd[:, 0:1], AF.Exp, reads=["rstd"], writes=["rstd"], scale=-0.5)

                with contextlib.ExitStack() as pe1:
                    wqb = k.sb(pe1, "wqb", [128, 16 * 2048], BF16)
                    sh2bc = k.sb(pe1, "sh2bc", [128, D], F32)
                    wm2bc = k.sb(pe1, "wm2bc", [128, D], F32)
                    kTb = k.sb(pe1, "kTb", [128, 256], BF16)
                    iota16 = k.sb(pe1, "iota16", [128, 16], F32)
                    k.dma("sp", sh2bc[:], rows_d[1, :, :], reads=["rows_d"], writes=["sh2bc"])
                    k.dma("sp", wm2bc[:], rows_d[2, :, :], reads=["rows_d"], writes=["wm2bc"])
                    k.dma("sp", iota16[:], iota16_in[:, :], writes=["iota16"])
                    stg = [k.sb(pe1, "stg%d" % i, [128, 2048], F32) for i in range(4)]
                    cnt = [0]
                    for kc in range(16):
                        load_cast(pe1, wqb[:, kc * 2048:(kc + 1) * 2048], "wqb", wq[kc * 128:(kc + 1) * 128, :], 2048,
                                  stg, ["stg%d" % i_ for i_ in range(4)], cnt)
                    load_cast(pe1, kTb[:, 0:128], "kTb", k1T[:, :], 128, stg, ["stg%d" % i_ for i_ in range(4)], cnt)
                    load_cast(pe1, kTb[:, 128:256], "kTb", k2T[:, :], 128, stg, ["stg%d" % i_ for i_ in range(4)], cnt)
                    k.barrier()
                    x1t = k.sb(pe1, "x1t", [128, D], F32)
                    h2 = k.sb(pe1, "h2", [128, D], F32)
                    h2b = k.sb(pe1, "h2b", [128, D], BF16)
                    h2T = k.sb(pe1, "h2T", [128, D], BF16)
                    qT = k.sb(pe1, "qT", [128, D], BF16)
                    s_r = [k.sb(pe1, "s_%d" % i, [128, D], F32) for i in range(2)]
                    s2 = [k.sb(pe1, "s2_%d" % i, [128, 256], F32) for i in range(4)]
                    V = k.sb(pe1, "V", [128, 256], F32)
                    I_ = k.sb(pe1, "I_", [128, 256], U32)
                    If = k.sb(pe1, "If", [128, 256], F32)
                    cand = k.sb(pe1, "cand", [128, D], F32)
                    OH = k.sb(pe1, "OH", [128, D], F32)
                    tops = k.sb(pe1, "tops", [128, 128], F32)
                    pos = k.sb(pe1, "pos", [128, 128], U32)
                    posf = k.sb(pe1, "posf", [128, 128], F32)
                    af = k.sb(pe1, "af", [128, 128], F32)
                    bf_ = k.sb(pe1, "bf_", [128, 128], F32)
                    ee = k.sb(pe1, "ee", [128, 128], F32)
                    Z = k.sb(pe1, "Z", [128, 8], F32)
                    I1s = k.sb(pe1, "I1s", [128, 128], F32)
                    I2s = k.sb(pe1, "I2s", [128, 128], F32)
                    eidf = k.sb(pe1, "eidf", [128, 128], F32)
                    pT2 = k.ps(pe1, "pT2", [128, D], BF16)
                    pq = k.ps(pe1, "pq", [128, D], F32)

                    def top16(src_ap, srck, vdst, idst, vk, ik, scratch, sk):
                        k.op("dve", lambda g_: g_.max(out=vdst[:, 0:8], in_=src_ap), reads=[srck], writes=[vk])
                        k.op("dve", lambda g_: g_.max_index(out=idst[:, 0:8], in_max=vdst[:, 0:8], in_values=src_ap),
                             reads=[srck, vk], writes=[ik])
                        k.op("dve", lambda g_: g_.match_replace(out=scratch, in_to_replace=vdst[:, 0:8], in_values=src_ap,
                                                                imm_value=NEG_BIG), reads=[srck, vk], writes=[sk])
                        k.op("dve", lambda g_: g_.max(out=vdst[:, 8:16], in_=scratch), reads=[sk], writes=[vk])
                        k.op("dve", lambda g_: g_.max_index(out=idst[:, 8:16], in_max=vdst[:, 8:16], in_values=scratch),
                             reads=[sk, vk], writes=[ik])

                    def top16_multi(items):
                        for (src, srck, vd, idt, vk, ik, scr, sk) in items:
                            k.op("dve", lambda g_: g_.max(out=vd[:, 0:8], in_=src), reads=[srck], writes=[vk])
                        for (src, srck, vd, idt, vk, ik, scr, sk) in items:
                            k.op("dve", lambda g_: g_.max_index(out=idt[:, 0:8], in_max=vd[:, 0:8], in_values=src),
                                 reads=[srck, vk], writes=[ik])
                        for (src, srck, vd, idt, vk, ik, scr, sk) in items:
                            k.op("dve", lambda g_: g_.match_replace(out=scr, in_to_replace=vd[:, 0:8], in_values=src,
                                                                    imm_value=NEG_BIG), reads=[srck, vk], writes=[sk])
                        for (src, srck, vd, idt, vk, ik, scr, sk) in items:
                            k.op("dve", lambda g_: g_.max(out=vd[:, 8:16], in_=scr), reads=[sk], writes=[vk + "b"])
                        for (src, srck, vd, idt, vk, ik, scr, sk) in items:
                            k.op("dve", lambda g_: g_.max_index(out=idt[:, 8:16], in_max=vd[:, 8:16], in_values=scr),
                                 reads=[sk, vk + "b"], writes=[ik + "b"])

                    def e1_stage1(tile_i):
                        r0 = tile_i * 128
                        s_ = s_r[tile_i % 2]
                        sk_ = "s_%d" % (tile_i % 2)
                        k.dma("sp", x1t[:], x1_d[r0:r0 + 128, :], reads=["x1_d"], writes=["x1t"])
                        rms_rstd(x1t, "x1t", h2b, "h2b")
                        k.act(h2[:], x1t[:], AF.Copy, reads=["x1t", "rstd"], writes=["h2"], scale=rstd[:, 0:1])
                        k.tt("pool", h2[:], h2[:], wm2bc[:], ALU.mult, reads=["h2", "wm2bc"], writes=["h2"])
                        k.tt("pool", h2[:], h2[:], sh2bc[:], ALU.add, reads=["h2", "sh2bc"], writes=["h2"])
                        k.act(h2b[:], h2[:], AF.Copy, reads=["h2"], writes=["h2b"])
                        k.dma("sp", h2b_d[r0:r0 + 128, :], h2b[:], reads=["h2b"], writes=["h2b_d"])
                        for kc in range(16):
                            k.tr(pT2[:, kc * 128:(kc + 1) * 128], h2b[:, kc * 128:(kc + 1) * 128], ident_b[:],
                                 reads=["h2b", "ident_b"], writes=["pT2"], sig=(kc == 15))
                        k.act(h2T[:], pT2[:], AF.Copy, reads=["pT2"], writes=["h2T"])
                        for f in range(16):
                            for kc in range(16):
                                k.mm(pq[:, f * 128:(f + 1) * 128], wqb[:, kc * 2048 + f * 128:kc * 2048 + (f + 1) * 128],
                                     h2T[:, kc * 128:(kc + 1) * 128], kc == 0, kc == 15, reads=["wqb", "h2T"], writes=["pq"],
                                     sig=(f == 15 and kc == 15))
                        k.act(qT[:], pq[:], AF.Copy, reads=["pq"], writes=["qT"])
                        for f in range(16):
                            k.mm(pq[:, f * 128:(f + 1) * 128], qT[:, f * 128:(f + 1) * 128],
                                 kTb[:, (f % 2) * 128:(f % 2 + 1) * 128], True, True, reads=["qT", "kTb"], writes=["pq"],
                                 sig=(f == 15))
                        k.act(s_[:], pq[:], AF.Copy, reads=["pq"], writes=[sk_])

                    def e1_stage2(tile_i):
                        eid = eid_all[:, tile_i * 128:(tile_i + 1) * 128]
                        gw = gw_all[:, tile_i * 128:(tile_i + 1) * 128]
                        s_ = s_r[tile_i % 2]
                        sk_ = "s_%d" % (tile_i % 2)
                        for f0 in range(0, 16, 4):
                            top16_multi([(s_[:, f * 128:(f + 1) * 128], sk_, V[:, f * 16:(f + 1) * 16],
                                          I_[:, f * 16:(f + 1) * 16], "V%d" % f, "I%d" % f, s2[f % 4][:, 0:128],
                                          "s2_%d" % (f % 4)) for f in range(f0, f0 + 4)])
                        Vkeys = ["V%d" % f for f in range(16)] + ["V%db" % f for f in range(16)]
                        Ikeys = ["I%d" % f for f in range(16)] + ["I%db" % f for f in range(16)]
                        k.tt("dve", mk_ap(cand, 0, [[256, 8], [16, 16], [1, 16]]), mk_ap(V, 0, [[32, 8], [1, 16], [0, 16]]),
                             mk_ap(V, 16, [[32, 8], [0, 16], [1, 16]]), ALU.add, reads=Vkeys, writes=["cand"])
                        for h0 in range(0, 8, 4):
                            top16_multi([(cand[:, h * 256:(h + 1) * 256], "cand", tops[:, h * 16:(h + 1) * 16],
                                          pos[:, h * 16:(h + 1) * 16], "tp%d" % h, "ps%d" % h, s2[h % 4][:, 0:256],
                                          "s2_%d" % (h % 4)) for h in range(h0, h0 + 4)])
                        Tkeys = ["tp%d" % h for h in range(8)] + ["tp%db" % h for h in range(8)]
                        Pkeys = ["ps%d" % h for h in range(8)] + ["ps%db" % h for h in range(8)]
                        k.tt("dve", mk_ap(ee, 0, [[16, 8], [1, 16]]), mk_ap(tops, 0, [[16, 8], [1, 16]]),
                             mk_ap(tops, 0, [[16, 8], [0, 16]]), ALU.subtract, reads=Tkeys, writes=["ee"])
                        k.act(ee[:], ee[:], AF.Exp, reads=["ee"], writes=["ee"])
                        k.op("dve", lambda g_: g_.tensor_reduce(out=Z[:], in_=mk_ap(ee, 0, [[16, 8], [1, 16]]),
                                                                axis=AX.X, op=ALU.add), reads=["ee"], writes=["Z"])
                        k.op("dve", lambda g_: g_.reciprocal(out=Z[:], in_=Z[:]), reads=["Z"], writes=["Z"])
                        k.tt("dve", mk_ap(gw, 0, [[16, 8], [1, 16]]), mk_ap(ee, 0, [[16, 8], [1, 16]]),
                             mk_ap(Z, 0, [[1, 8], [0, 16]]), ALU.mult, reads=["ee", "Z"], writes=["gw_all"])
                        k.cp("dve", posf[:], pos[:], reads=Pkeys, writes=["posf"])
                        k.cp("dve", If[:], I_[:], reads=Ikeys, writes=["If"])
                        k.ts("dve", af[:], posf[:], 1.0 / 16, -0.46875, ALU.mult, ALU.add, reads=["posf"], writes=["af"])
                        k.ts("dve", af[:], af[:], MAGIC, MAGIC, ALU.add, ALU.subtract, reads=["af"], writes=["af"])
                        k.stt(bf_[:], af[:], -16.0, posf[:], ALU.mult, ALU.add, reads=["af", "posf"], writes=["bf_"])
                        for (sel, src_off, dst, dk) in ((af, 0, I1s, "I1s"), (bf_, 16, I2s, "I2s")):
                            selk = "af" if sel is af else "bf_"
                            k.tt("dve", mk_ap(OH, 0, [[256, 8], [16, 16], [1, 16]]),
                                 mk_ap(sel, 0, [[16, 8], [1, 16], [0, 16]]),
                                 mk_ap(iota16, 0, [[0, 8], [0, 16], [1, 16]]), ALU.is_equal, reads=[selk, "iota16"],
                                 writes=["OH"])
                            k.tt("dve", mk_ap(OH, 0, [[256, 8], [16, 16], [1, 16]]),
                                 mk_ap(OH, 0, [[256, 8], [16, 16], [1, 16]]),
                                 mk_ap(If, src_off, [[32, 8], [0, 16], [1, 16]]), ALU.mult, reads=["OH", "If"], writes=["OH"])
                            k.op("dve", lambda g_: g_.tensor_reduce(out=dst[:], in_=mk_ap(OH, 0, [[16, 128], [1, 16]]),
                                                                    axis=AX.X, op=ALU.add), reads=["OH"], writes=[dk])
                        k.stt(eidf[:], I1s[:], 128.0, I2s[:], ALU.mult, ALU.add, reads=["I1s", "I2s"], writes=["eidf"])
                        k.cp("dve", eid, eidf[:], reads=["eidf"], writes=["eid_all"])

                    e1_stage1(0)
                    for tile_i in range(NT):
                        if tile_i + 1 < NT:
                            e1_stage1(tile_i + 1)
                        e1_stage2(tile_i)
                    k.barrier()

                with contextlib.ExitStack() as pe3:
                    g2bc = k.sb(pe3, "g2bc", [128, D], F32)
                    fwbc = k.sb(pe3, "fwbc", [128, D], F32)
                    k.dma("sp", g2bc[:], rows_d[3, :, :], reads=["rows_d"], writes=["g2bc"])
                    k.dma("sp", fwbc[:], fw_bc[:, :], writes=["fwbc"])
                    x1r = [k.sb(pe3, "x1r%d" % i, [128, D], F32) for i in range(2)]
                    h2r = [k.sb(pe3, "h2r%d" % i, [128, D], BF16) for i in range(2)]
                    junkb = k.sb(pe3, "junkb", [128, D], BF16)
                    junk2 = k.sb(pe3, "junk2", [128, D], BF16)
                    dots = k.sb(pe3, "dots", [128, 128], F32)
                    gel = k.sb(pe3, "gel", [128, 128], F32)
                    ot = k.sb(pe3, "ot", [128, D], F32)
                    NBUV, NDG = 8, 4
                    uvr = [k.sb(pe3, "uv%d" % i, [128, 2 * D], BF16) for i in range(NBUV)]
                    dgr = [k.sb(pe3, "dg%d" % i, [128, 128], BF16) for i in range(NDG)]
                    paccs = [k.ps(pe3, "pacc%d" % i, [128, D], F32) for i in range(2)]
                    cn = {"uv": 0, "d": 0}
                    for tile_i in range(NT):
                        r0 = tile_i * 128
                        par = tile_i % 2
                        x1t, x1k, h2t, h2k = x1r[par], "x1r%d" % par, h2r[par], "h2r%d" % par
                        pacc = paccs[par]
                        pkeys = ["pacc%d_%d" % (par, j) for j in range(4)]
                        k.dma("sp", x1t[:], x1_d[r0:r0 + 128, :], reads=["x1_d"], writes=[x1k])
                        k.dma("sp", h2t[:], h2b_d[r0:r0 + 128, :], reads=["h2b_d"], writes=[h2k])
                        pend = None
                        for slot in range(128):
                            uv, uvk = uvr[cn["uv"] % NBUV], "uv%d" % (cn["uv"] % NBUV)
                            cn["uv"] += 1
                            gcol = tile_i * 128 + slot
                            k.dma("pool", uv[:], uv16_d[:, :], reads=["eid_all", "u16_d", "v16_d"], writes=[uvk],
                                  idx=eid_all[:, gcol:gcol + 1])
                            k.stt(junkb[:], uv[:, 0:D], 1.0, h2t[:], ALU.mult, ALU.mult, reads=[uvk, h2k],
                                  writes=["junkb", "dots%d" % slot], accum_out=dots[:, slot:slot + 1])
                            k.act(gel[:, slot:slot + 1], dots[:, slot:slot + 1], AF.Gelu_apprx_tanh,
                                  reads=["dots%d" % slot], writes=["gel%d" % slot])
                            cur = (slot, uv, uvk, gcol)
                            for (sl_, uv_, uvk_, gcol_) in ([pend] if pend else []) + ([cur] if slot == 127 else []):
                                dg, dgk = dgr[cn["d"] % NDG], "dg%d" % (cn["d"] % NDG)
                                cn["d"] += 1
                                k.ts("dve", dg[:], ident_f[:], gel[:, sl_:sl_ + 1], gw_all[:, gcol_:gcol_ + 1],
                                     ALU.mult, ALU.mult, reads=["ident_f", "gel%d" % sl_, "gw_all"], writes=[dgk])
                                for nb in range(4):
                                    k.mm(pacc[:, nb * 512:(nb + 1) * 512], dg[:], uv_[:, D + nb * 512:D + (nb + 1) * 512],
                                         sl_ == 0, sl_ == 127, reads=[dgk, uvk_], writes=[pkeys[nb]], sig=(nb == 3))
                            pend = cur
                        k.tt("dve", ot[:], pacc[:], g2bc[:], ALU.mult, reads=pkeys + ["g2bc"], writes=["ot"])
                        k.tt("dve", ot[:], ot[:], x1t[:], ALU.add, reads=["ot", x1k], writes=["ot"])
                        rms_rstd(ot, "ot", junk2, "junk2")
                        k.stt(ot[:], ot[:], rstd[:, 0:1], fwbc[:], ALU.mult, ALU.mult, reads=["ot", "rstd", "fwbc"],
                              writes=["ot"])
                        k.dma("sp", y_out[r0:r0 + 128, :], ot[:], reads=["ot"], writes=["y_out"])
                    k.barrier()

        k.final_wait()
    return nc


def _pool_mats(flip):
    wins = (2, 4, 8, 16)
    out = np.zeros((128, 4, 128), np.float32)
    for g, w in enumerate(wins):
        P = np.zeros((64, 64), np.float32)
        for pos in range(64):
            lo = min(max(pos - w // 2, 0), 63)
            hi = min(max(pos + w // 2 - 1, 0), 63)
            P[lo:hi + 1, pos] += 1.0 / (hi - lo + 1)
            P[pos, pos] -= 1.0
        if flip:
            P = P[::-1, ::-1]
        out[0:64, g, 0:64] = P
        out[64:128, g, 64:128] = P
    return out.reshape(128, 512)


def _fm(v, n):
    return np.ascontiguousarray(np.asarray(v, np.float32).reshape(n, 128).T)


def prep_core(inp, b, half, last):
    flip = half == 1
    x = inp["x"][b]
    ctx = inp["ctx"][b]
    if flip:
        x = x[::-1]
        ctx = ctx[::-1]
    m = {}
    m["xs"] = np.ascontiguousarray(np.concatenate([x, ctx], axis=0), dtype=np.float32)
    m["cT"] = np.concatenate([_fm(inp["c"][b], 16), _fm(inp["c_ctx"], 16)], axis=1)
    m["w_mod"] = inp["w_mod"][0]
    m["bmod_bc"] = np.ascontiguousarray(np.broadcast_to(inp["b_mod"][0][None, :], (128, 6 * D)))
    m["n1T"] = _fm(inp["norm1_w"][0], 16)
    m["n2_bc"] = np.ascontiguousarray(np.broadcast_to(inp["norm2_w"][0][None, :], (128, D)))
    m["fw_bc"] = np.ascontiguousarray(np.broadcast_to(inp["final_w"][None, :], (128, D)))
    m["w_in"] = inp["w_in"][0]
    m["ident"] = np.eye(128, dtype=np.float32)
    m["pm2"] = _pool_mats(flip)
    m["poolw"] = np.ascontiguousarray(inp["pool_w"][0].reshape(1024, 256))
    m["pscT"] = _fm(inp["pool_scale"][0], 8)
    e0 = np.zeros((128, 1), np.float32)
    e0[0, 0] = 1.0
    m["e0"] = e0

    if last >= 2:
        dsel = [1, 0] if flip else [0, 1]
        a_re = inp["s5_a_re"][0]; a_im = inp["s5_a_im"][0]
        ldt = np.broadcast_to(inp["s5_log_dt"][0][:, :, None], (2, 64, 64))

        def part_layout(a):
            arr = np.stack([a[dsel[0]], a[dsel[1]]], 0)
            t_ = arr.transpose(2, 0, 1).reshape(64, 128)
            return np.ascontiguousarray(np.concatenate([t_, t_], 0), dtype=np.float32)

        def free_layout(a):
            arr = np.stack([a[dsel[0]], a[dsel[1]]], 0).reshape(2, 8, 8, 64)
            arr = arr.transpose(2, 1, 0, 3)
            arr = np.broadcast_to(arr[:, None, :, :, None, :], (8, 16, 8, 2, 2, 64))
            return np.ascontiguousarray(arr.reshape(128, 2048), dtype=np.float32)

        def b_layout(b0, b1):
            out = np.zeros((8, 16, 8, 2, 2, 64), np.float32)
            for slot in range(2):
                d = dsel[slot]
                for ri, arr in ((0, b0), (1, b1)):
                    a = arr[d].reshape(8, 8, 64, 16)
                    out[:, :, :, slot, ri, :] = a.transpose(1, 3, 0, 2)
            return out.reshape(128, 2048)

        def c_layout(c0, c1):
            out = np.zeros((2, 64, 2, 64, 16), np.float32)
            for slot in range(2):
                d = dsel[slot]
                out[0, :, slot] = c0[d].transpose(2, 0, 1)
                out[1, :, slot] = c1[d].transpose(2, 0, 1)
            return out.reshape(128, 2048)

        m["tidx"] = np.ascontiguousarray(np.broadcast_to(np.arange(TSEQ, dtype=np.float32)[None, :], (128, TSEQ)))
        m["s5_arep"] = part_layout(a_re); m["s5_aimp"] = part_layout(a_im); m["s5_ldtp"] = part_layout(ldt)
        m["s5_aref"] = free_layout(a_re); m["s5_aimf"] = free_layout(a_im); m["s5_ldtf"] = free_layout(ldt)
        bre = inp["s5_b_re"][0]; bim = inp["s5_b_im"][0]
        m["s5_brt"] = b_layout(bre, bim); m["s5_bst"] = b_layout(bim, bre)
        cre = inp["s5_c_re"][0]; cim = inp["s5_c_im"][0]
        m["s5_l1"] = c_layout(cre, cim); m["s5_l2"] = c_layout(cim, cre)
        sgn = np.zeros((128, 132), np.float32)
        sgn[:64, 0] = 1.0; sgn[64:, 0] = -1.0; sgn[:, 1] = -1.0
        sgn[:, 4:68] = -1.0; sgn[:, 68:132] = 1.0
        m["sgn"] = sgn
        gm = np.zeros((128, 8), np.float32)
        for gi in range(8):
            gm[gi * 16:(gi + 1) * 16, gi] = 1.0
        m["gmask"] = gm
        m["dskT"] = _fm(inp["s5_d"][0], 8)
        m["bgluT"] = _fm(inp["b_glu"][0], 8)
        m["w_glu"] = inp["w_glu"][0]
    if last >= 3:
        m["proj_a"] = inp["proj_a"][0]
        m["proj_b"] = inp["proj_b"][0]
        m["w_out"] = inp["w_out"][0]
    if last >= 4:
        m["wq"] = inp["peer_wq"][0]
        m["k1T"] = np.ascontiguousarray(inp["peer_k1"][0].T)
        m["k2T"] = np.ascontiguousarray(inp["peer_k2"][0].T)
        m["peer_u"] = inp["peer_u"][0]
        m["peer_v"] = inp["peer_v"][0]
        m["iota16"] = np.ascontiguousarray(np.broadcast_to(np.arange(16, dtype=np.float32)[None, :], (128, 16)))
    return m


_PROG = {}


def kernel(**inputs):
    inp = {k_: np.asarray(v) for k_, v in inputs.items()}
    if "nc" not in _PROG:
        _PROG["nc"] = build_program(stop_after="E", dbg=False)
    nc = _PROG["nc"]
    in_maps = []
    for core in range(8):
        b, half = core // 2, core % 2
        in_maps.append(prep_core(inp, b, half, 4))
    res = run_bass_kernel_spmd(nc, in_maps, core_ids=list(range(8)))
    out = np.empty((4, SEQ, D), np.float32)
    for core in range(8):
        b, half = core // 2, core % 2
        y = np.asarray(res.results[core]["y"], dtype=np.float32)
        if half == 0:
            out[b, 0:NOWN] = y
        else:
            out[b, NOWN:SEQ] = y[::-1]
    return out
```
